# Optimizing a Trainium2 kernel written in Bass

```python
import jax, jax.numpy as jnp
from jax import lax
import numpy as np

D_MODEL = 1024
BATCH = 32
SEQ = 2048
DEPTH = 1

CTX_LEN = 256
GRID_W = 64
NORM_EPS = 1e-6
SGU_WIDTH = 1024
SGU_GROUPS = 8
SGU_GROUP_DIM = SGU_WIDTH // SGU_GROUPS
CHUNK = 128
TINY_DIM = 64
ATTN_BLOCK = 128
ROPE_BASE = 10000.0
NA_HEADS = 16
NA_HEAD_DIM = 32
NA_WIDTH = NA_HEADS * NA_HEAD_DIM
NA_ROWS = 8
NA_COLS = 16
N_EXPERTS = 64
TOP_K = 8
EXPERT_FF = 256
SHARED_FF = 256
ROUTE_SCALE = 2.5
EXPERT_BLOCK = 128
COL_U = 0
COL_V = COL_U + SGU_WIDTH
COL_TQ = COL_V + SGU_WIDTH
COL_TK = COL_TQ + TINY_DIM
COL_TV = COL_TK + TINY_DIM
COL_NQ = COL_TV + TINY_DIM
COL_NK = COL_NQ + NA_WIDTH
COL_NV = COL_NK + NA_WIDTH
COL_GA = COL_NV + NA_WIDTH
COL_GB = COL_GA + D_MODEL
IN_WIDTH = COL_GB + D_MODEL

kernel_name = "hybrid_sgu_natten_moe_dit_block"


def rms_norm(x, g):
    xf = x.astype(jnp.float32)
    y = xf * lax.rsqrt(jnp.mean(xf * xf, -1, keepdims=True) + NORM_EPS)
    return (y * g.astype(jnp.float32)).astype(x.dtype)


def layer_norm(x, g, b):
    xf = x.astype(jnp.float32)
    mu = jnp.mean(xf, -1, keepdims=True)
    var = jnp.mean(jnp.square(xf - mu), -1, keepdims=True)
    y = (xf - mu) * lax.rsqrt(var + NORM_EPS) * g.astype(jnp.float32) + b.astype(jnp.float32)
    return y.astype(x.dtype)


def modulate(h, shift, scale):
    return h * (1 + scale) + shift


def swiglu(x, wg, wu, wd):
    return (jax.nn.silu(x @ wg) * (x @ wu)) @ wd


def axial_rope(x, n_tokens):
    dh = x.shape[-1]
    quarter = dh // 4
    t = jnp.arange(n_tokens)
    pos = jnp.stack([t // GRID_W, t % GRID_W], -1).astype(jnp.float32)
    inv_freq = ROPE_BASE ** (-jnp.arange(quarter, dtype=jnp.float32) / quarter)
    ang = pos[:, :, None] * inv_freq
    cos, sin = jnp.cos(ang), jnp.sin(ang)
    xf = x.astype(jnp.float32).reshape(x.shape[:-1] + (2, 2, quarter))
    x1, x2 = xf[..., 0, :], xf[..., 1, :]
    out = jnp.stack([x1 * cos - x2 * sin, x2 * cos + x1 * sin], -2)
    return out.reshape(x.shape).astype(x.dtype)


def tiny_attention(q, k_lat, v_lat, k_ctx, v_ctx):
    b, s, t = q.shape
    k_all = jnp.concatenate([k_lat, k_ctx], 1)
    v_all = jnp.concatenate([v_lat, v_ctx], 1)
    q_blocks = q.reshape(b, s // ATTN_BLOCK, ATTN_BLOCK, t).transpose(1, 0, 2, 3)
    scale = t ** -0.5

    def one_block(qb):
        sc = jnp.einsum("bqt,bkt->bqk", qb, k_all, preferred_element_type=jnp.float32) * scale
        p = jax.nn.softmax(sc, -1).astype(v_all.dtype)
        return jnp.einsum("bqk,bkt->bqt", p, v_all)

    out = lax.map(one_block, q_blocks)
    return out.transpose(1, 0, 2, 3).reshape(b, s, t)


def spatial_gating(u, v, tiny, ln_g, ln_b, w_s, b_s):
    b, s, _ = v.shape
    vn = layer_norm(v, ln_g, ln_b).reshape(b, s // CHUNK, CHUNK, SGU_GROUPS, SGU_GROUP_DIM)
    mixed = jnp.einsum("gpq,bnqgc->bnpgc", w_s, vn) + b_s.T[:, :, None]
    return u * (mixed.reshape(b, s, SGU_WIDTH) + tiny)


def neighbourhood_attention(q, k, v, k_ctx, v_ctx, rpb, rows):
    b, s, h, dh = q.shape
    kh = min(NA_ROWS, rows)
    qg = q.reshape(b, rows, GRID_W, h, dh)
    kg = k.reshape(b, rows, GRID_W, h, dh)
    vg = v.reshape(b, rows, GRID_W, h, dh)
    cq = np.arange(GRID_W)
    c_start = np.clip(cq - NA_COLS // 2, 0, GRID_W - NA_COLS)
    col_mask = (cq[None, :] >= c_start[:, None]) & (cq[None, :] < c_start[:, None] + NA_COLS)
    band_mask = np.broadcast_to(col_mask[:, None, :], (GRID_W, kh, GRID_W)).reshape(GRID_W, kh * GRID_W)
    dc_idx = np.clip(cq[None, :] - cq[:, None], -(NA_COLS - 1), NA_COLS - 1) + NA_COLS - 1
    col_bias = rpb[:, :, dc_idx]
    scale = dh ** -0.5

    def one_row(r):
        r_start = jnp.clip(r - kh // 2, 0, rows - kh)
        qb = lax.dynamic_index_in_dim(qg, r, axis=1, keepdims=False)
        kb = lax.dynamic_slice_in_dim(kg, r_start, kh, axis=1).reshape(b, kh * GRID_W, h, dh)
        vb = lax.dynamic_slice_in_dim(vg, r_start, kh, axis=1).reshape(b, kh * GRID_W, h, dh)
        dr = r_start + jnp.arange(kh) - r + NA_ROWS - 1
        bias = jnp.take(col_bias, dr, axis=1).transpose(0, 2, 1, 3).reshape(h, GRID_W, kh * GRID_W)
        s_lat = jnp.einsum("bqhd,bkhd->bhqk", qb, kb, preferred_element_type=jnp.float32) * scale + bias
        s_lat = jnp.where(band_mask, s_lat, -jnp.inf)
        s_ctx = jnp.einsum("bqhd,bkhd->bhqk", qb, k_ctx, preferred_element_type=jnp.float32) * scale
        p = jax.nn.softmax(jnp.concatenate([s_lat, s_ctx], -1), -1).astype(v.dtype)
        n_lat = kh * GRID_W
        return (jnp.einsum("bhqk,bkhd->bqhd", p[..., :n_lat], vb)
                + jnp.einsum("bhqk,bkhd->bqhd", p[..., n_lat:], v_ctx))

    out = lax.map(one_row, jnp.arange(rows))
    return out.transpose(1, 0, 2, 3, 4).reshape(b, s, h * dh)


def moe_ffn(h, w_router, b_router, w_e_gate, w_e_up, w_e_down, w_sh_gate, w_sh_up, w_sh_down):
    n, d = h.shape
    scores = jax.nn.sigmoid(jnp.matmul(h, w_router, preferred_element_type=jnp.float32))
    _, idx = lax.top_k(scores + b_router.astype(jnp.float32), TOP_K)
    wts = jnp.take_along_axis(scores, idx, axis=-1)
    wts = (wts / jnp.sum(wts, -1, keepdims=True) * ROUTE_SCALE).astype(h.dtype)
    n_assign = n * TOP_K
    flat_e = idx.reshape(-1)
    flat_tok = jnp.repeat(jnp.arange(n, dtype=jnp.int32), TOP_K)
    flat_w = wts.reshape(-1)
    order = jnp.argsort(flat_e)
    se, st, sw = flat_e[order], flat_tok[order], flat_w[order]
    counts = jnp.bincount(flat_e, length=N_EXPERTS)
    starts = jnp.cumsum(counts) - counts
    pad_counts = (counts + EXPERT_BLOCK - 1) // EXPERT_BLOCK * EXPERT_BLOCK
    pad_ends = jnp.cumsum(pad_counts)
    pad_starts = pad_ends - pad_counts
    dest = pad_starts[se] + jnp.arange(n_assign) - starts[se]
    n_blocks = -(-n_assign // EXPERT_BLOCK) + N_EXPERTS
    n_rows = n_blocks * EXPERT_BLOCK
    row_tok = jnp.full((n_rows,), n, jnp.int32).at[dest].set(st)
    row_w = jnp.zeros((n_rows,), h.dtype).at[dest].set(sw)
    block_e = jnp.minimum(jnp.searchsorted(pad_ends, jnp.arange(n_blocks) * EXPERT_BLOCK, side="right"),
                          N_EXPERTS - 1)
    h_pad = jnp.concatenate([h, jnp.zeros((1, d), h.dtype)], 0)

    def expert_block(acc, blk):
        tok, wt, e = blk
        yb = swiglu(h_pad[tok], w_e_gate[e], w_e_up[e], w_e_down[e]) * wt[:, None]
        return acc.at[tok].add(yb), None

    routed, _ = lax.scan(expert_block, jnp.zeros((n + 1, d), h.dtype),
                         (row_tok.reshape(n_blocks, EXPERT_BLOCK), row_w.reshape(n_blocks, EXPERT_BLOCK), block_e))
    return swiglu(h, w_sh_gate, w_sh_up, w_sh_down) + routed[:n]


def hybrid_layer(x, c, ctx, c_ctx, w_ada, b_ada, norm_mix_g, norm_ffn_g, w_in, b_in,
                 sgu_ln_g, sgu_ln_b, sgu_w_s, sgu_b_s, tiny_w_o, na_rpb, w_up_a, w_up_b, w_out,
                 w_router, b_router, w_e_gate, w_e_up, w_e_down, w_sh_gate, w_sh_up, w_sh_down):
    b, s, d = x.shape
    rows = s // GRID_W
    ctx_len = ctx.shape[1]
    mod = (jax.nn.silu(c) @ w_ada + b_ada)[:, None, :]
    sh_mix, sc_mix, g_mix, sh_ffn, sc_ffn, g_ffn = jnp.split(mod, 6, -1)
    mod_ctx = jax.nn.silu(c_ctx) @ w_ada[:, :2 * d] + b_ada[:2 * d]

    h = modulate(rms_norm(x, norm_mix_g), sh_mix, sc_mix)
    p_in = h @ w_in + b_in
    u = jax.nn.gelu(p_in[..., COL_U:COL_V], approximate=False)
    v = jax.nn.gelu(p_in[..., COL_V:COL_TQ], approximate=False)
    tq = axial_rope(p_in[..., COL_TQ:COL_TK], s)
    tk = axial_rope(p_in[..., COL_TK:COL_TV], s)
    tv = p_in[..., COL_TV:COL_NQ]
    nq = p_in[..., COL_NQ:COL_NK].reshape(b, s, NA_HEADS, NA_HEAD_DIM)
    nk = p_in[..., COL_NK:COL_NV].reshape(b, s, NA_HEADS, NA_HEAD_DIM)
    nv = p_in[..., COL_NV:COL_GA].reshape(b, s, NA_HEADS, NA_HEAD_DIM)
    gate_a = jax.nn.sigmoid(p_in[..., COL_GA:COL_GB])
    gate_b = jax.nn.sigmoid(p_in[..., COL_GB:IN_WIDTH])

    hc = modulate(rms_norm(ctx, norm_mix_g), mod_ctx[:d], mod_ctx[d:])
    c_tiny = hc @ w_in[:, COL_TK:COL_NQ] + b_in[COL_TK:COL_NQ]
    tk_c, tv_c = c_tiny[..., :TINY_DIM], c_tiny[..., TINY_DIM:]
    c_na = hc @ w_in[:, COL_NK:COL_GA] + b_in[COL_NK:COL_GA]
    nk_c = c_na[..., :NA_WIDTH].reshape(b, ctx_len, NA_HEADS, NA_HEAD_DIM)
    nv_c = c_na[..., NA_WIDTH:].reshape(b, ctx_len, NA_HEADS, NA_HEAD_DIM)

    tiny = tiny_attention(tq, tk, tv, tk_c, tv_c) @ tiny_w_o
    o_a = spatial_gating(u, v, tiny, sgu_ln_g, sgu_ln_b, sgu_w_s, sgu_b_s)
    o_b = neighbourhood_attention(nq, nk, nv, nk_c, nv_c, na_rpb, rows)

    merged = gate_a * (o_a @ w_up_a) + gate_b * (o_b @ w_up_b)
    x = x + g_mix * (merged @ w_out)

    h2 = modulate(rms_norm(x, norm_ffn_g), sh_ffn, sc_ffn).reshape(b * s, d)
    y = moe_ffn(h2, w_router, b_router, w_e_gate, w_e_up, w_e_down, w_sh_gate, w_sh_up, w_sh_down)
    return x + g_ffn * y.reshape(b, s, d)


def setup_inputs(seed: int = 0) -> dict:
    key = jax.random.key(seed)
    ks = jax.random.split(key, 32)
    f32 = jnp.float32
    L, D = DEPTH, D_MODEL

    def nrm(k, shape, scale):
        return jax.random.normal(k, shape, f32) * scale

    return {
        "x": nrm(ks[0], (BATCH, SEQ, D), 1.0),
        "c": nrm(ks[1], (BATCH, D), 1.0),
        "ctx": nrm(ks[2], (BATCH, CTX_LEN, D), 1.0),
        "c_ctx": nrm(ks[3], (D,), 1.0),
        "w_ada": nrm(ks[4], (L, D, 6 * D), 0.5 * D ** -0.5),
        "b_ada": nrm(ks[5], (L, 6 * D), 0.01),
        "norm_mix_g": 1.0 + nrm(ks[6], (L, D), 0.1),
        "norm_ffn_g": 1.0 + nrm(ks[7], (L, D), 0.1),
        "w_in": nrm(ks[8], (L, D, IN_WIDTH), D ** -0.5),
        "b_in": nrm(ks[9], (L, IN_WIDTH), 0.01),
        "sgu_ln_g": 1.0 + nrm(ks[10], (L, SGU_WIDTH), 0.1),
        "sgu_ln_b": nrm(ks[11], (L, SGU_WIDTH), 0.01),
        "sgu_w_s": nrm(ks[12], (L, SGU_GROUPS, CHUNK, CHUNK), CHUNK ** -0.5),
        "sgu_b_s": 1.0 + nrm(ks[13], (L, SGU_GROUPS, CHUNK), 0.1),
        "tiny_w_o": nrm(ks[14], (L, TINY_DIM, SGU_WIDTH), TINY_DIM ** -0.5),
        "na_rpb": nrm(ks[15], (L, NA_HEADS, 2 * NA_ROWS - 1, 2 * NA_COLS - 1), 0.1),
        "w_up_a": nrm(ks[16], (L, SGU_WIDTH, D), SGU_WIDTH ** -0.5),
        "w_up_b": nrm(ks[17], (L, NA_WIDTH, D), NA_WIDTH ** -0.5),
        "w_out": nrm(ks[18], (L, D, D), D ** -0.5),
        "w_router": nrm(ks[19], (L, D, N_EXPERTS), D ** -0.5),
        "b_router": nrm(ks[20], (L, N_EXPERTS), 0.01),
        "w_e_gate": nrm(ks[21], (L, N_EXPERTS, D, EXPERT_FF), D ** -0.5),
        "w_e_up": nrm(ks[22], (L, N_EXPERTS, D, EXPERT_FF), D ** -0.5),
        "w_e_down": nrm(ks[23], (L, N_EXPERTS, EXPERT_FF, D), EXPERT_FF ** -0.5),
        "w_sh_gate": nrm(ks[24], (L, D, SHARED_FF), D ** -0.5),
        "w_sh_up": nrm(ks[25], (L, D, SHARED_FF), D ** -0.5),
        "w_sh_down": nrm(ks[26], (L, SHARED_FF, D), SHARED_FF ** -0.5),
        "final_norm_g": 1.0 + nrm(ks[27], (D,), 0.1),
    }


def reference(x, c, ctx, c_ctx, w_ada, b_ada, norm_mix_g, norm_ffn_g, w_in, b_in,
              sgu_ln_g, sgu_ln_b, sgu_w_s, sgu_b_s, tiny_w_o, na_rpb, w_up_a, w_up_b, w_out,
              w_router, b_router, w_e_gate, w_e_up, w_e_down, w_sh_gate, w_sh_up, w_sh_down,
              final_norm_g):
    for i in range(DEPTH):
        x = hybrid_layer(x, c, ctx, c_ctx, w_ada[i], b_ada[i], norm_mix_g[i], norm_ffn_g[i], w_in[i], b_in[i],
                         sgu_ln_g[i], sgu_ln_b[i], sgu_w_s[i], sgu_b_s[i], tiny_w_o[i], na_rpb[i],
                         w_up_a[i], w_up_b[i], w_out[i], w_router[i], b_router[i],
                         w_e_gate[i], w_e_up[i], w_e_down[i], w_sh_gate[i], w_sh_up[i], w_sh_down[i])
    return rms_norm(x, final_norm_g)
```

```python
import numpy as np
from contextlib import ExitStack
import concourse.bass as bass
import concourse.mybir as mybir
from concourse.bass_utils import run_bass_kernel_spmd

F32 = mybir.dt.float32
BF16 = mybir.dt.bfloat16
AF = mybir.ActivationFunctionType
ALU = mybir.AluOpType

import os as _os0
USE_POW = int(_os0.environ.get("KPOW", "1"))
SEM_LIMIT = 20000
N_DMA_SEMS = 40

D = 1024
SEQ = 2048
CTX = 256
NKEY = SEQ + CTX
NE = 64
EPS = 1e-6
NA_SCALE = 32 ** -0.5
TINY_SCALE = 64 ** -0.5
NEG = -30000.0


class Sched:
    ENGS = ("pe", "act", "dve", "pool", "sp")

    def __init__(self, nc, es, same_engine_sync=("pool",), same_engine_raw=("act", "dve", "pool")):
        self.nc = nc
        self.es = es
        self.ins = []
        self.state = {}
        self.same = set(same_engine_sync)
        self.same_raw = set(same_engine_raw)
        self.bar = {e: set() for e in self.ENGS}
        self.last = {}
        self.dmas_since_bar = []
        self.dma_hist = []
        self._tok = 0

    def tok(self):
        self._tok += 1
        return self._tok

    def op(self, eng, fn, r=(), w=(), dma=False):
        idx = len(self.ins)
        key = ("dma", idx) if dma else eng
        deps = set()
        force = dma or (eng in self.same)
        force_raw = force or (eng in self.same_raw)
        for t in r:
            st = self.state.setdefault(t, [{}, {}])
            for k, i in st[0].items():
                if k != key or force_raw:
                    deps.add(i)
        for t in w:
            st = self.state.setdefault(t, [{}, {}])
            for k, i in st[0].items():
                if k != key or force:
                    deps.add(i)
            for k, i in st[1].items():
                if k != key or force:
                    deps.add(i)
        for t in r:
            self.state[t][1][key] = idx
        for t in w:
            self.state[t] = [{key: idx}, {}]
        if self.bar[eng]:
            deps |= self.bar[eng]
            self.bar[eng] = set()
        if dma:
            if len(self.dma_hist) >= N_DMA_SEMS:
                deps.add(self.dma_hist[-N_DMA_SEMS])
            self.dma_hist.append(idx)
            self.dmas_since_bar.append(idx)
        deps.discard(idx)
        self.ins.append(dict(eng=eng, fn=fn, deps=deps, dma=dma))
        self.last[eng] = idx
        return idx

    def barrier(self):
        s = set(self.last.values()) | set(self.dmas_since_bar)
        for e in self.ENGS:
            self.bar[e] |= s
        self.dmas_since_bar = []

    def emit(self):
        nc, es = self.nc, self.es
        needed = set()
        for ins in self.ins:
            needed |= ins["deps"]
        eng_sems = {e: [] for e in self.ENGS}
        eng_cnt = {e: SEM_LIMIT for e in self.ENGS}
        dma_sems = [es.enter_context(nc.semaphore(f"dq{i}")) for i in range(N_DMA_SEMS)]
        nd = 0
        for i, ins in enumerate(self.ins):
            if ins["dma"]:
                ins["sem"] = ("d", nd % N_DMA_SEMS)
                ins["val"] = 16 * (nd // N_DMA_SEMS + 1)
                ins["semh"] = dma_sems[nd % N_DMA_SEMS]
                nd += 1
            elif i in needed:
                e = ins["eng"]
                if eng_cnt[e] >= SEM_LIMIT:
                    eng_sems[e].append(es.enter_context(nc.semaphore(f"s_{e}{len(eng_sems[e])}")))
                    eng_cnt[e] = 0
                eng_cnt[e] += 1
                ins["sem"] = (e, len(eng_sems[e]) - 1)
                ins["val"] = eng_cnt[e]
                ins["semh"] = eng_sems[e][-1]
        progs = {e: [] for e in self.ENGS}
        waited = {e: {} for e in self.ENGS}
        for i, ins in enumerate(self.ins):
            e = ins["eng"]
            waits = {}
            for d in ins["deps"]:
                p = self.ins[d]
                if "sem" not in p:
                    continue
                sk = p["sem"]
                if sk[0] != "d":
                    cur = waited[e].get(("E", sk[0]), (-1, 0))
                    if (sk[1], p["val"]) <= cur:
                        continue
                    prev = waits.get(("E", sk[0]))
                    if prev is None or (sk[1], p["val"]) > (prev[0], prev[1]):
                        waits[("E", sk[0])] = (sk[1], p["val"], p["semh"])
                else:
                    cur = waited[e].get(sk, 0)
                    if p["val"] <= cur:
                        continue
                    prev = waits.get(sk)
                    if prev is None or p["val"] > prev[1]:
                        waits[sk] = (0, p["val"], p["semh"])
            wl = []
            for k, (ep, v, h) in waits.items():
                waited[e][k] = (ep, v) if k[0] == "E" else v
                wl.append((h, v))
            progs[e].append((wl, ins["fn"], ins.get("semh"), 16 if ins["dma"] else 1))
        self.stats = {e: len(progs[e]) for e in self.ENGS}

        def run(eng, prog):
            for wl, fn, semh, n in prog:
                for h, v in wl:
                    eng.wait_ge(h, v)
                if fn is None:
                    continue
                r = fn(eng)
                if semh is not None:
                    r.then_inc(semh, n)

        block = es.enter_context(nc.Block())

        @block.tensor
        def _(eng):
            run(eng, progs["pe"])

        @block.scalar
        def _(eng):
            run(eng, progs["act"])

        @block.vector
        def _(eng):
            run(eng, progs["dve"])

        @block.gpsimd
        def _(eng):
            run(eng, progs["pool"])

        @block.sync
        def _(eng):
            run(eng, progs["sp"])


class B:
    def __init__(self, S, t):
        self.t = t
        self.k = S.tok()


class K:
    def __init__(self, nc, S):
        self.nc, self.S = nc, S

    def _nm(self, name):
        self._n = getattr(self, "_n", 0) + 1
        return f"{name}_{self._n}"

    def sb(self, es, name, shape, dt):
        return B(self.S, es.enter_context(self.nc.sbuf_tensor(self._nm(name), shape, dt)))

    def ps(self, es, name, shape, dt=F32):
        return B(self.S, es.enter_context(self.nc.psum_tensor(self._nm(name), shape, dt)))

    def mm(self, out, lhsT, rhs, start, stop, r, w):
        self.S.op("pe", lambda e: e.matmul(out, lhsT=lhsT, rhs=rhs, start=start, stop=stop), r=r, w=w)

    def tr(self, out, in_, ident, r, w):
        self.S.op("pe", lambda e: e.transpose(out=out, in_=in_, identity=ident), r=r, w=w)

    def act(self, out, in_, func, r, w, bias=None, scale=None, accum=None):
        kw = {}
        if bias is not None:
            kw["bias"] = bias
        if scale is not None:
            kw["scale"] = scale
        if accum is not None:
            kw["accum_out"] = accum
        self.S.op("act", lambda e: e.activation(out=out, in_=in_, func=func, **kw), r=r, w=w)

    def tt(self, eng, out, in0, in1, op, r, w):
        self.S.op(eng, lambda e: e.tensor_tensor(out=out, in0=in0, in1=in1, op=op), r=r, w=w)

    def ts(self, eng, out, in0, s1, op0, r, w, s2=None, op1=None):
        if op1 is None:
            self.S.op(eng, lambda e: e.tensor_scalar(out=out, in0=in0, scalar1=s1, scalar2=None, op0=op0), r=r, w=w)
        else:
            self.S.op(eng, lambda e: e.tensor_scalar(out=out, in0=in0, scalar1=s1, scalar2=s2, op0=op0, op1=op1), r=r, w=w)

    def stt(self, out, in0, scalar, in1, op0, op1, r, w):
        self.S.op("dve", lambda e: e.scalar_tensor_tensor(out=out, in0=in0, scalar=scalar, in1=in1, op0=op0, op1=op1), r=r, w=w)

    def cp(self, eng, out, in_, r, w):
        if eng == "act":
            self.S.op("act", lambda e: e.copy(out=out, in_=in_), r=r, w=w)
        else:
            self.S.op(eng, lambda e: e.tensor_copy(out=out, in_=in_), r=r, w=w)

    def dma(self, eng, out, in_, r, w):
        self.S.op(eng, lambda e: e.dma_start(out=out, in_=in_), r=r, w=w, dma=True)

    def rstd2(self, tmp, ss, out, rk, wk, scale):
        if USE_POW:
            self.S.op("dve", lambda e: e.tensor_scalar(out=tmp, in0=ss, scalar1=scale, scalar2=EPS, op0=ALU.mult, op1=ALU.add), r=rk, w=wk)
            self.S.op("pool", lambda e: e.tensor_tensor(out=out, in0=tmp, in1=self.mhalf.t[:, 0:1], op=ALU.pow), r=wk + [self.mhalf.k], w=wk)
        else:
            self.act(tmp, ss, AF.Sqrt, r=rk, w=wk, bias=self.epsb.t[:, 0:1], scale=scale)
            self.S.op("dve", lambda e: e.reciprocal(out=out, in_=tmp), r=wk, w=wk)

    def rstd(self, es_bufs, ss, out, r_k, scale):
        tmp = es_bufs
        self.act(tmp.t[:, 0:1], ss, AF.Sqrt, r=r_k, w=[tmp.k], bias=self.epsb.t[:, 0:1], scale=scale)
        self.S.op("dve", lambda e: e.reciprocal(out=out[0], in_=tmp.t[:, 0:1]), r=[tmp.k], w=[out[1]])


def build(NB=4, debug=None):
    nc = bass.Bass("TRN2", target_bir_lowering=False)
    NT = NB * SEQ

    def din(name, shape, dt=F32):
        return nc.dram_tensor(name, list(shape), dt, kind="ExternalInput").ap()

    x = din("x", [NB, SEQ, D])
    ctx = din("ctx", [NB, CTX, D])
    cT = din("cT", [128, 8, 5])
    w_ada = din("w_ada", [D, 6 * D])
    b_ada = din("b_ada", [1, 6 * D])
    nmg = din("nmg", [128, 8])
    nfg = din("nfg", [128, 8])
    w_in = din("w_in", [D, 5824 + 256])
    b_u = din("b_u", [128, 8])
    b_ga = din("b_ga", [128, 8])
    b_gb = din("b_gb", [128, 8])
    b_nq = din("b_nq", [128, 4])
    b_nk = din("b_nk", [128, 4])
    b_nv = din("b_nv", [128, 4])
    b_t4 = din("b_t4", [128, 2])
    b_tv = din("b_tv", [64, 1])
    b_v = din("b_v", [1, D])
    ln_g = din("ln_g", [1, D])
    ln_b = din("ln_b", [1, D])
    w_s = din("w_s", [8, 128, 128])
    b_s = din("b_s", [1, D])
    tiny_w_o = din("tiny_w_o", [64, D])
    nab2 = din("nab2", [4, 9, 128, 4, 128])
    nmk2 = din("nmk2", [9, 128, 128])
    cs = din("cs", [128, SEQ])
    hmask = din("hmask", [128, 4])
    stacki = din("stacki", [128, 64])
    eplace = din("eplace", [32, 4, 128])
    w_up_a = din("w_up_a", [D, D])
    w_up_b = din("w_up_b", [512, D])
    w_out = din("w_out", [D, D])
    w_router = din("w_router", [D, NE])
    b_router = din("b_router", [1, NE])
    w_gu = din("w_gu", [NE + 1, D, 512])
    w_d = din("w_d", [NE + 1, 256, D])
    fng = din("fng", [1, D])
    y = nc.dram_tensor("y", [NB, SEQ, D], F32, kind="ExternalOutput").ap()

    kM = "ExternalOutput" if debug == "M" else ("ExternalInput" if debug == "E" else "Internal")
    x1s = nc.dram_tensor("x1s", [NT, D], F32, kind=kM).ap()
    h2s = nc.dram_tensor("h2s", [NB, 128, 8, SEQ], BF16, kind=kM).ap()
    Gs = nc.dram_tensor("Gs", [NT, NE], F32, kind=kM).ap()
    gsc = nc.dram_tensor("gsc", [NB, 2, D], F32, kind=kM).ap()
    hTs = nc.dram_tensor("hTs", [NB, 4, 128, 8, 512], BF16, kind="Internal").ap()
    EBs = nc.dram_tensor("EBs", [128, 4, 9, 512], BF16, kind="Internal").ap()

    es = ExitStack()
    with es:
        S = Sched(nc, es)
        kb = K(nc, S)
        sb, ps, mm, tr, act, tt, ts, stt, cp, dma = kb.sb, kb.ps, kb.mm, kb.tr, kb.act, kb.tt, kb.ts, kb.stt, kb.cp, kb.dma
        T_y = S.tok()
        T_x1s, T_h2s, T_Gs, T_gsc, T_hTs = S.tok(), S.tok(), S.tok(), S.tok(), S.tok()
        T_EBs = S.tok()

        ident = sb(es, "ident", [128, 128], F32)
        identb = sb(es, "identb", [128, 128], BF16)
        epsb = sb(es, "epsb", [128, 1], F32)
        kb.epsb = epsb
        S.op("pool", lambda e: e.memset(ident.t[:], 0.0), w=[ident.k])
        S.op("pool", lambda e: e.affine_select(out=ident.t[:], in_=ident.t[:], pattern=[[-1, 128]], compare_op=ALU.not_equal,
                                               fill=1.0, base=0, channel_multiplier=1), r=[ident.k], w=[ident.k])
        cp("dve", identb.t[:], ident.t[:], r=[ident.k], w=[identb.k])
        S.op("dve", lambda e: e.memset(epsb.t[:], EPS), w=[epsb.k])
        mhalf = sb(es, "mhalf", [128, 1], F32)
        kb.mhalf = mhalf
        S.op("pool", lambda e: e.memset(mhalf.t[:], -0.5), w=[mhalf.k])
        fng_bc = sb(es, "fng_bc", [128, D], F32)
        dma("sp", fng_bc.t[:], fng.to_broadcast([128, D]), r=[], w=[fng_bc.k])

        if debug != "E":
            phase_m(nc, S, kb, es, locals())
        S.barrier()
        if debug != "M":
            phase_e(nc, S, kb, es, locals())
        S.op("sp", None, r=[T_y, T_x1s, T_h2s, T_Gs, T_gsc])
        S.emit()
    return nc


def phase_e(nc, S, kb, es0, L):
    sb, ps, mm, tr, act, tt, ts, stt, cp, dma = kb.sb, kb.ps, kb.mm, kb.tr, kb.act, kb.tt, kb.ts, kb.stt, kb.cp, kb.dma
    NB = L["NB"]
    x1s, h2s, Gs, gsc, y, w_gu, w_d = L["x1s"], L["h2s"], L["Gs"], L["gsc"], L["y"], L["w_gu"], L["w_d"]
    T_y, T_x1s, T_h2s, T_Gs, T_gsc = L["T_y"], L["T_x1s"], L["T_h2s"], L["T_Gs"], L["T_gsc"]
    identb, fng_bc = L["identb"], L["fng_bc"]
    es = ExitStack()
    with es:
        acc = sb(es, "acc", [128, 16, D], F32)
        acck = [S.tok() for _ in range(16)]
        h2T = sb(es, "h2T", [128, 8, SEQ], BF16)
        Gt = [sb(es, f"Gt{i}", [128, 16, NE], F32) for i in range(2)]
        gffn = [sb(es, f"gffn{i}", [128, D], F32) for i in range(2)]
        wgu = [sb(es, f"wgu{i}", [128, 8, 512], BF16) for i in range(4)]
        wd = [sb(es, f"wd{i}", [128, 2, D], BF16) for i in range(4)]
        sg = [sb(es, f"sg{i}", [128, 256], F32) for i in range(2)]
        a_ = [sb(es, f"a{i}", [128, 256], BF16) for i in range(2)]
        aT = [sb(es, f"aT{i}", [128, 2, 128], BF16) for i in range(2)]
        x1t = [sb(es, f"x1t{i}", [128, D], F32) for i in range(4)]
        ssk = [S.tok() for _ in range(4)]
        ot = [sb(es, f"ot{i}", [128, D], F32) for i in range(2)]
        junk = sb(es, "junkE", [128, D], BF16)
        ssE = sb(es, "ssE", [128, 8], F32)
        pY = [ps(es, f"pY{i}", [128, 2, 512]) for i in range(2)]
        pGU = [ps(es, f"pGU{i}", [128, 512]) for i in range(2)]
        pT = [ps(es, f"pT{i}", [128, 2, 128], BF16) for i in range(2)]

        pairs = [(2 * i, 2 * i + 1) for i in range(NE // 2)] + [(NE,)]
        NP = len(pairs)
        wslots = {}
        wcount = [0]

        def issue_loads(k):
            if k >= NB * NP or k in wslots:
                return
            pair = pairs[k % NP]
            wsl = {}
            for e in pair:
                sl = wcount[0] % 4
                wcount[0] += 1
                wsl[e] = sl
                dma("pool", wgu[sl].t[:], w_gu[e].rearrange("(k p) n -> p k n", p=128), r=[], w=[wgu[sl].k])
                dma("pool", wd[sl].t[:], w_d[e].rearrange("(j p) n -> p j n", p=128), r=[], w=[wd[sl].k])
            wslots[k] = wsl

        def load_h2T(b):
            dma("sp", h2T.t[:], h2s[b], r=[T_h2s], w=[h2T.k])

        def load_gates(b):
            dma("sp", Gt[b % 2].t[:], Gs[b * SEQ:(b + 1) * SEQ, :].rearrange("(t p) e -> p t e", p=128), r=[T_Gs], w=[Gt[b % 2].k])
            dma("sp", gffn[b % 2].t[:], gsc[b, 1:2, :].to_broadcast([128, D]), r=[T_gsc], w=[gffn[b % 2].k])

        def final_stage(b, t, st, tail=False):
            s4 = t % 4
            row0 = b * SEQ + t * 128
            gf = gffn[b % 2]
            c = 2 * s4
            xt = x1t[s4]
            if st == 0:
                dma("sp", xt.t[:], x1s[row0:row0 + 128, :], r=[T_x1s], w=[xt.k])
                tt("dve" if tail else "pool", acc.t[:, t, :], acc.t[:, t, :], gf.t[:], ALU.mult, r=[acck[t], gf.k], w=[acck[t]])
                tt("dve", xt.t[:], xt.t[:], acc.t[:, t, :], ALU.add, r=[xt.k, acck[t]], w=[xt.k])
            elif st == 1:
                S.op("dve", lambda e: e.memset(ssE.t[:, c:c + 1], 0.0), w=[ssk[s4]])
                act(junk.t[:], xt.t[:], AF.Square, r=[xt.k], w=[junk.k, ssk[s4]], accum=ssE.t[:, c:c + 1])
            elif st == 2:
                kb.rstd2(ssE.t[:, c + 1:c + 2], ssE.t[:, c:c + 1], ssE.t[:, c:c + 1], [ssk[s4]], [ssk[s4]], 1.0 / D)
            else:
                o_ = ot[t % 2]
                stt(o_.t[:], xt.t[:], ssE.t[:, c:c + 1], fng_bc.t[:], ALU.mult, ALU.mult,
                    r=[xt.k, ssk[s4], fng_bc.k], w=[o_.k])
                dma("sp", y[b, t * 128:(t + 1) * 128, :], o_.t[:], r=[o_.k], w=[T_y])

        def final_tile(b, t):
            for st in range(4):
                final_stage(b, t, st, tail=True)

        load_h2T(0)
        load_gates(0)
        issue_loads(0)
        for b in range(NB):
            Gb = Gt[b % 2]
            for pi, pair in enumerate(pairs):
                kglob = b * NP + pi
                issue_loads(kglob + 1)
                wsl = wslots[kglob]
                if pi == 1 and b + 1 < NB:
                    load_gates(b + 1)
                items = [(t, e) for t in range(16) for e in pair]
                n = len(items)

                def GU(i):
                    t, e = items[i]
                    s = i % 2
                    for k in range(8):
                        mm(pGU[s].t[:, :], h2T.t[:, k, t * 128:(t + 1) * 128], wgu[wsl[e]].t[:, k, :], k == 0, k == 7,
                           r=[h2T.k, wgu[wsl[e]].k], w=[pGU[s].k])

                def EW(i):
                    t, e = items[i]
                    s = i % 2
                    act(sg[s].t[:], pGU[s].t[:, 0:256], AF.Silu, r=[pGU[s].k], w=[sg[s].k])
                    gsc_ = 1.0 if e == NE else Gb.t[:, t, e:e + 1]
                    stt(a_[s].t[:], sg[s].t[:], gsc_, pGU[s].t[:, 256:512], ALU.mult, ALU.mult,
                        r=[sg[s].k, Gb.k, pGU[s].k], w=[a_[s].k])

                def TR(i):
                    s = i % 2
                    for j in range(2):
                        tr(pT[s].t[:, j, :], a_[s].t[:, j * 128:(j + 1) * 128], identb.t[:], r=[a_[s].k, identb.k], w=[pT[s].k])
                    cp("act", aT[s].t[:], pT[s].t[:], r=[pT[s].k], w=[aT[s].k])

                def DN(i):
                    t, e = items[i]
                    s = i % 2
                    ys = t % 2
                    first = (e == pair[0])
                    last = (e == pair[-1])
                    for nh in range(2):
                        for j in range(2):
                            mm(pY[ys].t[:, nh, :], aT[s].t[:, j, :], wd[wsl[e]].t[:, j, nh * 512:(nh + 1) * 512],
                               first and j == 0, last and j == 1, r=[aT[s].k, wd[wsl[e]].k], w=[pY[ys].k])
                    if last:
                        if pi == 0:
                            cp("dve", acc.t[:, t, :], pY[ys].t[:].rearrange("p a b -> p (a b)"), r=[pY[ys].k], w=[acck[t]])
                        else:
                            tt("dve", acc.t[:, t, :], acc.t[:, t, :], pY[ys].t[:].rearrange("p a b -> p (a b)"), ALU.add,
                               r=[pY[ys].k, acck[t]], w=[acck[t]])

                GU(0)
                for i in range(n):
                    if b > 0 and pi < 2:
                        g_ = pi * 32 + i
                        for st in (3, 2, 1, 0):
                            tq2 = g_ - st
                            if tq2 >= 0 and tq2 % 2 == 0 and tq2 // 2 < 16:
                                final_stage(b - 1, tq2 // 2, st)
                    if i + 1 < n:
                        GU(i + 1)
                    EW(i)
                    TR(i)
                    if i >= 1:
                        DN(i - 1)
                DN(n - 1)
            if b + 1 < NB:
                load_h2T(b + 1)
        for g_ in range(36):
            for st in (3, 2, 1, 0):
                tq2 = g_ - st
                if tq2 >= 0 and tq2 % 2 == 0 and tq2 // 2 < 16:
                    final_stage(NB - 1, tq2 // 2, st, tail=True)
        S.barrier()


class _Stop(Exception):
    pass


import os as _os
STOP = int(_os.environ.get("KSTOP", "0"))
EVAC_ACT_ONLY = int(_os.environ.get("KEVAC", "0"))
NOLD = int(_os.environ.get("KNOLD", "0"))
NWST = int(_os.environ.get("KNWST", "5"))
KSS = int(_os.environ.get("KSS", "0"))
NAD = int(_os.environ.get("KNAD", "2"))


def phase_m(nc, S, kb, es0, L):
    _phase_m(nc, S, kb, es0, L)
    S.barrier()


def _stop(n):
    return STOP == n


def _phase_m(nc, S, kb, es0, L):
    sb, ps, mm, tr, act, tt, ts, stt, cp, dma = kb.sb, kb.ps, kb.mm, kb.tr, kb.act, kb.tt, kb.ts, kb.stt, kb.cp, kb.dma
    NB = L["NB"]
    x, ctx, w_in = L["x"], L["ctx"], L["w_in"]
    x1s, h2s, Gs, gsc, hTs = L["x1s"], L["h2s"], L["Gs"], L["gsc"], L["hTs"]
    T_x1s, T_h2s, T_Gs, T_gsc, T_hTs = L["T_x1s"], L["T_h2s"], L["T_Gs"], L["T_gsc"], L["T_hTs"]
    ident, identb, epsb = L["ident"], L["identb"], L["epsb"]
    AXX = mybir.AxisListType.X
    esM = ExitStack()
    with esM:
        def load(es, name, shape, dt, src, eng="sp"):
            b_ = sb(es, name, shape, dt)
            dma(eng, b_.t[:], src, r=[], w=[b_.k])
            return b_

        def kcols(a, b_):
            return w_in[:, a:b_].rearrange("(k p) n -> p k n", p=128)

        nmg_t = load(esM, "nmg_t", [128, 8], F32, L["nmg"])
        nfg_t = load(esM, "nfg_t", [128, 8], F32, L["nfg"])
        b_u = load(esM, "b_u_t", [128, 8], F32, L["b_u"])
        b_ga = load(esM, "b_ga_t", [128, 8], F32, L["b_ga"])
        b_gb = load(esM, "b_gb_t", [128, 8], F32, L["b_gb"])
        b_nq = load(esM, "b_nq_t", [128, 4], F32, L["b_nq"])
        b_nk = load(esM, "b_nk_t", [128, 4], F32, L["b_nk"])
        b_nv = load(esM, "b_nv_t", [128, 4], F32, L["b_nv"])
        b_t4 = load(esM, "b_t4_t", [128, 2], F32, L["b_t4"])
        b_tv = load(esM, "b_tv_t", [64, 1], F32, L["b_tv"])
        bv_bc = load(esM, "bv_bc", [128, D], F32, L["b_v"].to_broadcast([128, D]))
        lng_bc = load(esM, "lng_bc", [128, D], F32, L["ln_g"].to_broadcast([128, D]))
        brt_bc = load(esM, "brt_bc", [128, NE], F32, L["b_router"].to_broadcast([128, NE]))
        CS = load(esM, "CS", [128, SEQ], F32, L["cs"])
        hmask = load(esM, "hmask_t", [128, 4], F32, L["hmask"])
        stacki = load(esM, "stacki_t", [128, 64], F32, L["stacki"])
        wr = load(esM, "wr", [128, 8, NE], F32, L["w_router"].rearrange("(k p) n -> p k n", p=128))
        two = load(esM, "two", [64, D], BF16, L["tiny_w_o"], eng="pool")
        epl = load(esM, "epl", [32, 4, 128], BF16, L["eplace"], eng="pool")
        ones_b = sb(esM, "ones_b", [128, 64], BF16)
        S.op("dve", lambda e: e.memset(ones_b.t[:], 1.0), w=[ones_b.k])
        modT = sb(esM, "modT", [128, 48, 5], F32)
        A1 = sb(esM, "A1", [128, 8, 5], F32)
        A2 = sb(esM, "A2", [128, 8, 5], F32)
        w_sT = sb(esM, "w_sT", [128, 8, 128], BF16)
        LB = sb(esM, "LB", [2, D], F32)
        RB = sb(esM, "RB", [2, D], F32)

        with ExitStack() as e1:
            scT = load(e1, "scT", [128, 8, 5], F32, L["cT"])
            act(scT.t[:], scT.t[:], AF.Silu, r=[scT.k], w=[scT.k])
            modrow = sb(e1, "modrow", [5, 6 * D], F32)
            bada = load(e1, "bada", [5, 6 * D], F32, L["b_ada"].to_broadcast([5, 6 * D]))
            wa = [sb(e1, f"wa{i}", [128, 8, 512], F32) for i in range(2)]
            pm = ps(e1, "pm", [128, 512])
            pmt = ps(e1, "pmt", [128, 48, 5])
            for cb in range(12):
                s_ = cb % 2
                dma("sp", wa[s_].t[:], L["w_ada"][:, cb * 512:(cb + 1) * 512].rearrange("(k p) n -> p k n", p=128), r=[], w=[wa[s_].k])
                for k in range(8):
                    mm(pm.t[0:5, :], scT.t[:, k, :], wa[s_].t[:, k, :], k == 0, k == 7, r=[scT.k, wa[s_].k], w=[pm.k])
                tt("dve", modrow.t[:, cb * 512:(cb + 1) * 512], pm.t[0:5, :], bada.t[:, cb * 512:(cb + 1) * 512], ALU.add,
                   r=[pm.k, bada.k], w=[modrow.k])
            for t in range(48):
                tr(pmt.t[:, t, :], modrow.t[0:5, t * 128:(t + 1) * 128], ident.t[0:5, 0:5], r=[modrow.k, ident.k], w=[pmt.k])
            cp("dve", modT.t[:], pmt.t[:], r=[pmt.k], w=[modT.k])
            for j in range(5):
                stt(A1.t[:, :, j], modT.t[:, 8:16, j], 1.0, nmg_t.t[:], ALU.add, ALU.mult, r=[modT.k, nmg_t.k], w=[A1.k])
                stt(A2.t[:, :, j], modT.t[:, 32:40, j], 1.0, nfg_t.t[:], ALU.add, ALU.mult, r=[modT.k, nfg_t.k], w=[A2.k])
            for b in range(NB):
                dma("sp", gsc[b, 0:1, :], modrow.t[b:b + 1, 2 * D:3 * D], r=[modrow.k], w=[T_gsc])
                dma("sp", gsc[b, 1:2, :], modrow.t[b:b + 1, 5 * D:6 * D], r=[modrow.k], w=[T_gsc])
            if _stop(1):
                return
            wsn = load(e1, "wsn", [128, 8, 128], F32, L["w_s"].rearrange("g p q -> p g q"))
            pw = [ps(e1, f"pw{i}", [128, 4, 128]) for i in range(2)]
            for g in range(8):
                tr(pw[g // 4].t[:, g % 4, :], wsn.t[:, g, :], ident.t[:], r=[wsn.k, ident.k], w=[pw[g // 4].k])
            for i in range(2):
                cp("dve", w_sT.t[:, 4 * i:4 * i + 4, :], pw[i].t[:], r=[pw[i].k], w=[w_sT.k])
            prs = ps(e1, "prs", [128, 2, 512])
            for g in range(8):
                mm(prs.t[0:1, g // 4, (g % 4) * 128:(g % 4 + 1) * 128], ones_b.t[:, 0:1], w_sT.t[:, g, :], True, True,
                   r=[ones_b.k, w_sT.k], w=[prs.k])
            cp("dve", RB.t[0:1, :], prs.t[0:1, :, :].rearrange("p a b -> p (a b)"), r=[prs.k], w=[RB.k])
            dma("sp", RB.t[1:2, :], L["b_s"], r=[], w=[RB.k])
            S.op("dve", lambda e: e.memset(LB.t[:], 1.0), w=[LB.k])
            dma("sp", LB.t[0:1, :], L["ln_b"], r=[], w=[LB.k])
        S.barrier()
        if _stop(2):
            return

        for b in range(NB):
            with ExitStack() as eb:
                tkT = sb(eb, "tkT", [64, NKEY], BF16)
                tva = sb(eb, "tva", [128, 18, 65], BF16)
                S.op("dve", lambda e: e.memset(tva.t[:, :, 64:65], 1.0), w=[tva.k])
                tqT = sb(eb, "tqT", [64, SEQ], BF16)
                obT = sb(eb, "obT", [128, 4, SEQ], BF16)
                gmix = sb(eb, "gmix", [128, D], F32)
                dma("sp", gmix.t[:], gsc[b, 0:1, :].to_broadcast([128, D]), r=[T_gsc], w=[gmix.k])
                with ExitStack() as e1:
                    nkT = sb(e1, "nkT", [128, 4, NKEY], BF16)
                    nva = sb(e1, "nva", [128, 18, 16, 33], BF16)
                    S.op("dve", lambda e: e.memset(nva.t[:, :, :, 32:33], 1.0), w=[nva.k])
                    nqT = sb(e1, "nqT", [128, 4, SEQ], BF16)
                    with ExitStack() as e1a:
                        wk1 = sb(e1a, "wk1", [128, 8, 1856], BF16)
                        for q_ in range(3):
                            dma("pool", wk1.t[:, :, q_ * 512:(q_ + 1) * 512], kcols(2240 + q_ * 512, 2240 + (q_ + 1) * 512), r=[], w=[wk1.k])
                        dma("pool", wk1.t[:, :, 1536:1792], kcols(5824, 6080), r=[], w=[wk1.k])
                        dma("pool", wk1.t[:, :, 1792:1856], kcols(2176, 2240), r=[], w=[wk1.k])
                        xs = [sb(e1a, f"xs{i}", [128, D], F32) for i in range(4)]
                        xn = [sb(e1a, f"xn{i}", [128, D], F32) for i in range(4)]
                        junk = sb(e1a, "junk1", [128, D], BF16)
                        ss = sb(e1a, "ss1", [128, 8], F32)
                        ssk1 = [S.tok() for _ in range(4)]
                        hT = [sb(e1a, f"hT{i}", [128, 8, 512], BF16) for i in range(2)]
                        hk = [[S.tok(), S.tok()] for _ in range(2)]
                        rbuf = sb(e1a, "rbuf", [128, 512], F32)
                        pX = [ps(e1a, f"pX{i}", [128, 4, 128]) for i in range(4)]
                        pP = [ps(e1a, f"pP{i}", [128, 512]) for i in range(3)]
                        p64 = ps(e1a, "p64", [128, 512])
                        pcnt = 0
                        tile_id = {}
                        for u_ in range(5):
                            for tl_ in range(4 if u_ < 4 else 2):
                                tile_id[(u_, tl_)] = len(tile_id)

                        def T1(u):
                            for tl in range(4 if u < 4 else 2):
                                g_ = tile_id[(u, tl)]
                                s_ = g_ % 4
                                src = x[b, u * 512 + tl * 128:u * 512 + (tl + 1) * 128, :] if u < 4 else ctx[b, tl * 128:(tl + 1) * 128, :]
                                dma("sp", xs[s_].t[:], src, r=[], w=[xs[s_].k])
                                c = 2 * s_
                                S.op("dve", lambda e, c=c: e.memset(ss.t[:, c:c + 1], 0.0), w=[ssk1[s_]])
                                act(junk.t[:], xs[s_].t[:], AF.Square, r=[xs[s_].k], w=[junk.k, ssk1[s_]], accum=ss.t[:, c:c + 1])
                                kb.rstd2(ss.t[:, c + 1:c + 2], ss.t[:, c:c + 1], ss.t[:, c:c + 1], [ssk1[s_]], [ssk1[s_]], 1.0 / D)
                                act(xn[s_].t[:], xs[s_].t[:], AF.Identity, r=[xs[s_].k, ssk1[s_]], w=[xn[s_].k], scale=ss.t[:, c:c + 1])

                        def T2(u):
                            j = b if u < 4 else 4
                            h = hT[u % 2]
                            hkk = hk[u % 2]
                            for tl in range(4 if u < 4 else 2):
                                g_ = tile_id[(u, tl)]
                                s_ = g_ % 4
                                p2 = g_ % 2
                                for kk in range(8):
                                    pb = pX[2 * p2 + kk // 4]
                                    tr(pb.t[:, kk % 4, :], xn[s_].t[:, kk * 128:(kk + 1) * 128], ident.t[:], r=[xn[s_].k, ident.k], w=[pb.k])
                                for kk in range(8):
                                    pb = pX[2 * p2 + kk // 4]
                                    o_ = h.t[:, kk, tl * 128:(tl + 1) * 128]
                                    if kk < 4:
                                        act(o_, pb.t[:, kk % 4, :], AF.Identity, r=[pb.k, A1.k, modT.k], w=[hkk[0]],
                                            scale=A1.t[:, kk, j:j + 1], bias=modT.t[:, kk, j:j + 1])
                                    else:
                                        stt(o_, pb.t[:, kk % 4, :], A1.t[:, kk, j:j + 1], modT.t[:, kk, j:j + 1].to_broadcast([128, 128]),
                                            ALU.mult, ALU.add, r=[pb.k, A1.k, modT.k], w=[hkk[1]])

                        T1(0)
                        T2(0)
                        for u in range(5):
                            ntile = 4 if u < 4 else 2
                            N = ntile * 128
                            h = hT[u % 2]
                            hkk = hk[u % 2]
                            if u + 1 < 5:
                                T1(u + 1)
                            if _stop(31):
                                return
                            if u < 4:
                                dma("sp", hTs[b, u], h.t[:], r=hkk, w=[T_hTs])
                                tsl = slice(u * 512, u * 512 + N)
                            else:
                                tsl = slice(SEQ, SEQ + N)

                            def proj_fm(c0, Mn=128):
                                nonlocal pcnt
                                P = pP[pcnt % 3]
                                pcnt += 1
                                for kk in range(8):
                                    mm(P.t[0:Mn, 0:N], wk1.t[:, kk, c0:c0 + Mn], h.t[:, kk, 0:N], kk == 0, kk == 7, r=[wk1.k] + hkk, w=[P.k])
                                return P

                            if _stop(32):
                                return
                            for g in range(4):
                                P = proj_fm(512 + g * 128)
                                act(nkT.t[:, g, tsl], P.t[:, 0:N], AF.Identity, r=[P.k, b_nk.k], w=[nkT.k], bias=b_nk.t[:, g:g + 1])
                            if _stop(33):
                                return
                            P = proj_fm(1536 + 128)
                            if u < 4:
                                stt(rbuf.t[:, 0:N], P.t[:, 0:N], b_t4.t[:, 1:2], CS.t[:, tsl], ALU.add, ALU.mult, r=[P.k, b_t4.k, CS.k], w=[rbuf.k])
                                mm(p64.t[0:64, 0:N], stacki.t[:], rbuf.t[:, 0:N], True, True, r=[stacki.k, rbuf.k], w=[p64.k])
                                cp("act", tkT.t[:, tsl], p64.t[0:64, 0:N], r=[p64.k], w=[tkT.k])
                                for g in range(4):
                                    P = proj_fm(g * 128)
                                    act(nqT.t[:, g, tsl], P.t[:, 0:N], AF.Identity, r=[P.k, b_nq.k], w=[nqT.k], bias=b_nq.t[:, g:g + 1])
                                P = proj_fm(1536)
                                stt(rbuf.t[:, 0:N], P.t[:, 0:N], b_t4.t[:, 0:1], CS.t[:, tsl], ALU.add, ALU.mult, r=[P.k, b_t4.k, CS.k], w=[rbuf.k])
                                mm(p64.t[0:64, 0:N], stacki.t[:], rbuf.t[:, 0:N], True, True, r=[stacki.k, rbuf.k], w=[p64.k])
                                cp("act", tqT.t[:, tsl], p64.t[0:64, 0:N], r=[p64.k], w=[tqT.k])
                            else:
                                act(tkT.t[:, tsl], P.t[0:64, 0:N], AF.Identity, r=[P.k, b_t4.k], w=[tkT.k], bias=b_t4.t[0:64, 1:2])
                            if _stop(34):
                                return
                            for tl in range(ntile):
                                kt = (u * 4 + tl) if u < 4 else 16 + tl
                                P = pP[pcnt % 3]
                                pcnt += 1
                                for kk in range(8):
                                    mm(P.t[:, :], h.t[:, kk, tl * 128:(tl + 1) * 128], wk1.t[:, kk, 1024:1536], kk == 0, kk == 7, r=[wk1.k] + hkk, w=[P.k])
                                cp("dve", nva.t[:, kt, :, 0:32], P.t[:, :].rearrange("p (h d) -> p h d", h=16), r=[P.k], w=[nva.k])
                                P = pP[pcnt % 3]
                                pcnt += 1
                                for kk in range(8):
                                    mm(P.t[:, 0:64], h.t[:, kk, tl * 128:(tl + 1) * 128], wk1.t[:, kk, 1792:1856], kk == 0, kk == 7, r=[wk1.k] + hkk, w=[P.k])
                                cp("act", tva.t[:, kt, 0:64], P.t[:, 0:64], r=[P.k], w=[tva.k])
                            if u + 1 < 5:
                                T2(u + 1)
                    S.barrier()
                    if _stop(3):
                        return
                    with ExitStack() as e1b:
                        EB = sb(e1b, "EB", [128, 4, 9, 512], BF16)
                        nst = [sb(e1b, f"nst{i}", [128, 4, 128], F32) for i in range(2)]
                        nmk = load(e1b, "nmk", [128, 9, 128], F32, L["nmk2"].rearrange("t k q -> k t q"))
                        if b == 0:
                            for g in range(4):
                                for ty in range(9):
                                    s_ = (g * 9 + ty) % 2
                                    dma("sp", nst[s_].t[:], L["nab2"][g, ty], r=[], w=[nst[s_].k])
                                    for hh in range(4):
                                        tt("dve", nst[s_].t[:, hh, :], nst[s_].t[:, hh, :], nmk.t[:, ty, :], ALU.add, r=[nst[s_].k, nmk.k], w=[nst[s_].k])
                                    act(EB.t[:, g, ty, :], nst[s_].t[:].rearrange("p a b -> p (a b)"), AF.Exp, r=[nst[s_].k], w=[EB.k])
                            if NB > 1:
                                dma("sp", L["EBs"], EB.t[:], r=[EB.k], w=[L["T_EBs"]])
                        else:
                            dma("sp", EB.t[:], L["EBs"], r=[L["T_EBs"]], w=[EB.k])
                        nqm = [sb(e1b, f"nqm{i}", [128, 4, 128], BF16) for i in range(NAD)]
                        Pn = [sb(e1b, f"Pn{i}", [128, 7, 512], BF16) for i in range(NAD)]
                        Pk = [[S.tok() for _ in range(7)] for _ in range(NAD)]
                        on = sb(e1b, "on", [128, 128], BF16)
                        rec = sb(e1b, "rec", [128, 4], F32)
                        pS = [ps(e1b, f"pS{i}", [128, 512]) for i in range(4)]
                        pO = [ps(e1b, f"pO{i}", [128, 512]) for i in range(2)]
                        pTr = ps(e1b, "pTr", [128, 1024], BF16)
                        scnt = [0]

                        def tiles(i):
                            if 2 <= i <= 13:
                                lat = [(i - 2, 7), (i - 1, 2), (i, 3), (i + 1, 4), (i + 2, 8)]
                            elif i < 2:
                                lat = [(j_, j_ - i + 3) for j_ in range(4)]
                            else:
                                lat = [(j_, j_ - i + 3) for j_ in range(12, 16)]
                            return lat + [(16, None), (17, None)]

                        def QK(n):
                            i, g = n // 4, n % 4
                            nq_ = nqm[n % NAD]
                            for hh in range(4):
                                ts("dve", nq_.t[:, hh, :], nqT.t[:, g, i * 128:(i + 1) * 128], hmask.t[:, hh:hh + 1], ALU.mult,
                                   r=[nqT.k, hmask.k], w=[nq_.k])
                            for ti, (kt, ty) in enumerate(tiles(i)):
                                S_ = pS[scnt[0] % 4]
                                scnt[0] += 1
                                mm(S_.t[:, :], nkT.t[:, g, kt * 128:(kt + 1) * 128], nq_.t[:].rearrange("p a b -> p (a b)"), True, True,
                                   r=[nkT.k, nq_.k], w=[S_.k])
                                pk = Pk[n % NAD][ti]
                                act(Pn[n % NAD].t[:, ti, :], S_.t[:, :], AF.Exp, r=[S_.k], w=[pk], scale=NA_SCALE)
                                if ty is not None:
                                    tt("dve", Pn[n % NAD].t[:, ti, :], Pn[n % NAD].t[:, ti, :], EB.t[:, g, ty, :], ALU.mult, r=[pk, EB.k], w=[pk])

                        def PV(n):
                            i, g = n // 4, n % 4
                            tl_ = tiles(i)
                            nt = len(tl_)
                            pO_ = pO[n % 2]
                            for hh in range(4):
                                h_ = 4 * g + hh
                                for ti, (kt, ty) in enumerate(tl_):
                                    mm(pO_.t[:, hh * 33:(hh + 1) * 33], Pn[n % NAD].t[:, ti, hh * 128:(hh + 1) * 128], nva.t[:, kt, h_, :], ti == 0, ti == nt - 1,
                                       r=[Pk[n % NAD][ti], nva.k], w=[pO_.k])
                            S.op("dve", lambda e: e.reciprocal(out=rec.t[:, 0:4], in_=pO_.t[:, 0:132].rearrange("p (h d) -> p h d", h=4)[:, :, 32]),
                                 r=[pO_.k], w=[rec.k])
                            for hh in range(4):
                                ts("dve", on.t[:, hh * 32:(hh + 1) * 32], pO_.t[:, hh * 33:hh * 33 + 32], rec.t[:, hh:hh + 1], ALU.mult,
                                   r=[pO_.k, rec.k], w=[on.k])
                            tr(pTr.t[:, 0:128], on.t[:], identb.t[:], r=[on.k, identb.k], w=[pTr.k])
                            act(obT.t[:, g, i * 128:(i + 1) * 128], pTr.t[:, 0:128], AF.Identity, r=[pTr.k, b_nv.k], w=[obT.k], bias=b_nv.t[:, g:g + 1])

                        for n in range(NAD - 1):
                            QK(n)
                        for n in range(64):
                            if n + NAD - 1 < 64:
                                QK(n + NAD - 1)
                            PV(n)
                    S.barrier()
                if _stop(4):
                    return
                with ExitStack() as e2:
                    wst = [sb(e2, f"wst{i}", [128, 8, 512], BF16) for i in range(NWST)]
                    wub = sb(e2, "wub", [128, 4, D], BF16)
                    dma("pool", wub.t[:], L["w_up_b"].rearrange("(k p) n -> p k n", p=128), r=[], w=[wub.k])
                    wcnt = [0]

                    def ld(src):
                        wb_ = wst[wcnt[0] % NWST]
                        wcnt[0] += 1
                        if not (NOLD and wcnt[0] > NWST):
                            dma("pool", wb_.t[:], src, r=[], w=[wb_.k])
                        return wb_

                    uT = sb(e2, "uT", [128, 8, 512], BF16)
                    mgT = sb(e2, "mgT", [128, 8, 512], BF16)
                    hT2 = [sb(e2, f"hT2{i}", [128, 8, 512], BF16) for i in range(2)]
                    xs2 = [sb(e2, f"xs2{i}", [128, D], F32) for i in range(2)]
                    vb = sb(e2, "vb", [128, D], F32)
                    vhat = [sb(e2, f"vhat{i}", [128, D], BF16) for i in range(2)]
                    st6 = sb(e2, "st6", [128, 2, 6], F32)
                    mv = sb(e2, "mv", [128, 4], F32)
                    Ptc = [sb(e2, f"Ptc{i}", [128, 18, 128], BF16) for i in range(2)]
                    Ptk = [[S.tok() for _ in range(5)] for _ in range(2)]
                    rc1 = sb(e2, "rc1", [128, 2], F32)
                    tn = sb(e2, "tn", [128, 64], F32)
                    tinyT = [sb(e2, f"tinyT{i}", [64, 128], BF16) for i in range(2)]
                    gab = [sb(e2, f"gab{i}", [128, 512], F32) for i in range(2)]
                    t12 = [sb(e2, f"t12{i}", [128, 512], F32) for i in range(2)]
                    tD = sb(e2, "tD", [128, D], F32)
                    xn2 = sb(e2, "xn2", [128, D], F32)
                    junk2 = sb(e2, "junk2", [128, D], BF16)
                    ss2 = sb(e2, "ss2", [128, 4], F32)
                    h2f2 = [sb(e2, f"h2f{i}", [128, 8, 128], F32) for i in range(2)]
                    h2fk2 = [[S.tok(), S.tok()] for _ in range(2)]
                    h2b = sb(e2, "h2b", [128, 8, 128], BF16)
                    rt2 = [sb(e2, f"rt{i}", [128, 5, NE], F32) for i in range(2)]
                    m82 = [sb(e2, f"m8{i}", [128, 16], F32) for i in range(2)]
                    Q = [ps(e2, f"Q{i}", [128, 512]) for i in range(8)]
                    q4 = lambda q_: q_.t[:, :].rearrange("p (a b) -> p a b", a=4)
                    for s_ in range(4):
                        h = hT2[s_ % 2]
                        dma("sp", h.t[:], hTs[b, s_], r=[T_hTs], w=[h.k])
                        for half in range(2):
                            wb_ = ld(kcols(half * 512, (half + 1) * 512))
                            for f4 in range(4):
                                ft = half * 4 + f4
                                P = Q[ft % 2]
                                for kk in range(8):
                                    mm(P.t[:, :], wb_.t[:, kk, f4 * 128:(f4 + 1) * 128], h.t[:, kk, :], kk == 0, kk == 7, r=[wb_.k, h.k], w=[P.k])
                                act(uT.t[:, ft, :], P.t[:, :], AF.Gelu, r=[P.k, b_u.k], w=[uT.k], bias=b_u.t[:, ft:ft + 1])
                        if _stop(5):
                            return
                        wv = [ld(kcols(1024, 1536)), ld(kcols(1536, 2048))]

                        def XB(c):
                            tq_ = s_ * 4 + c
                            sl_ = c % 2
                            for half in range(2):
                                P = Q[half]
                                for kk in range(8):
                                    mm(P.t[:, :], h.t[:, kk, c * 128:(c + 1) * 128], wv[half].t[:, kk, :], kk == 0, kk == 7, r=[h.k, wv[half].k], w=[P.k])
                                tt("dve", vb.t[:, half * 512:(half + 1) * 512], P.t[:, :], bv_bc.t[:, half * 512:(half + 1) * 512], ALU.add,
                                   r=[P.k, bv_bc.k], w=[vb.k])
                            for grp in range(5):
                                kts = list(range(4 * grp, min(4 * grp + 4, 18)))
                                pSt = Q[2 + grp % 2]
                                for jj, kt in enumerate(kts):
                                    mm(q4(pSt)[:, jj, :], tkT.t[:, kt * 128:(kt + 1) * 128], tqT.t[:, tq_ * 128:(tq_ + 1) * 128], True, True,
                                       r=[tkT.k, tqT.k], w=[pSt.k])
                                act(Ptc[sl_].t[:, kts[0]:kts[-1] + 1, :], q4(pSt)[:, 0:len(kts), :], AF.Exp, r=[pSt.k], w=[Ptk[sl_][grp]], scale=TINY_SCALE)
                            act(vb.t[:], vb.t[:], AF.Gelu, r=[vb.k], w=[vb.k])
                            for i2 in range(2):
                                S.op("dve", lambda e, i2=i2: e.bn_stats(out=st6.t[:, i2, :], in_=vb.t[:, i2 * 512:(i2 + 1) * 512]), r=[vb.k], w=[st6.k])
                            S.op("dve", lambda e: e.bn_aggr(out=mv.t[:, 0:2], in_=st6.t[:].rearrange("p a b -> p (a b)")), r=[st6.k], w=[mv.k])
                            kb.rstd2(mv.t[:, 2:3], mv.t[:, 1:2], mv.t[:, 3:4], [mv.k], [mv.k], 1.0)
                            stt(vb.t[:], vb.t[:], mv.t[:, 0:1], lng_bc.t[:], ALU.subtract, ALU.mult, r=[vb.k, mv.k, lng_bc.k], w=[vb.k])
                            act(vhat[sl_].t[:], vb.t[:], AF.Identity, r=[vb.k, mv.k], w=[vhat[sl_].k], scale=mv.t[:, 3:4])
                            for kt in range(18):
                                mm(Q[4].t[:, 0:65], Ptc[sl_].t[:, kt, :], tva.t[:, kt, :], kt == 0, kt == 17, r=[Ptk[sl_][kt // 4], tva.k], w=[Q[4].k])
                            S.op("dve", lambda e: e.reciprocal(out=rc1.t[:, 0:1], in_=Q[4].t[:, 64:65]), r=[Q[4].k], w=[rc1.k])
                            ts("dve", tn.t[:], Q[4].t[:, 0:64], rc1.t[:, 0:1], ALU.mult, r=[Q[4].k, rc1.k], w=[tn.k])
                            tr(Q[5].t[0:64, 0:128], tn.t[:], ident.t[:], r=[tn.k, ident.k], w=[Q[5].k])
                            act(tinyT[sl_].t[:], Q[5].t[0:64, 0:128], AF.Identity, r=[Q[5].k, b_tv.k], w=[tinyT[sl_].k], bias=b_tv.t[:, 0:1])

                        def YB(c):
                            sl_ = c % 2
                            for ft in range(8):
                                bank = Q[6 + ft // 4]
                                sl = q4(bank)[:, ft % 4, :]
                                mm(sl, vhat[sl_].t[:, ft * 128:(ft + 1) * 128], w_sT.t[:, ft, :], True, False, r=[vhat[sl_].k, w_sT.k], w=[bank.k])
                                mm(sl, two.t[:, ft * 128:(ft + 1) * 128], tinyT[sl_].t[:], False, False, r=[two.k, tinyT[sl_].k], w=[bank.k])
                                mm(sl, LB.t[:, ft * 128:(ft + 1) * 128], RB.t[:, ft * 128:(ft + 1) * 128], False, True, r=[LB.k, RB.k], w=[bank.k])
                            for j2 in range(2):
                                tt("dve", uT.t[:, 4 * j2:4 * j2 + 4, c * 128:(c + 1) * 128], uT.t[:, 4 * j2:4 * j2 + 4, c * 128:(c + 1) * 128],
                                   q4(Q[6 + j2]), ALU.mult, r=[uT.k, Q[6 + j2].k], w=[uT.k])

                        XB(0)
                        for c in range(4):
                            if c + 1 < 4:
                                XB(c + 1)
                            YB(c)
                        if _stop(6):
                            return
                        for half in range(2):
                            wa_ = ld(L["w_up_a"][:, half * 512:(half + 1) * 512].rearrange("(k p) n -> p k n", p=128))
                            wga = ld(kcols(3776 + half * 512, 3776 + (half + 1) * 512))
                            wgb = ld(kcols(4800 + half * 512, 4800 + (half + 1) * 512))
                            for f4 in range(4):
                                ft = half * 4 + f4
                                cs_ = slice(f4 * 128, (f4 + 1) * 128)
                                o4 = 4 * (ft % 2)
                                pA, pB, pGa, pGb = Q[o4], Q[o4 + 1], Q[o4 + 2], Q[o4 + 3]
                                for kk in range(8):
                                    mm(pGa.t[:, :], wga.t[:, kk, cs_], h.t[:, kk, :], kk == 0, kk == 7, r=[wga.k, h.k], w=[pGa.k])
                                for kk in range(8):
                                    mm(pGb.t[:, :], wgb.t[:, kk, cs_], h.t[:, kk, :], kk == 0, kk == 7, r=[wgb.k, h.k], w=[pGb.k])
                                for kk in range(8):
                                    mm(pA.t[:, :], wa_.t[:, kk, cs_], uT.t[:, kk, :], kk == 0, kk == 7, r=[wa_.k, uT.k], w=[pA.k])
                                for kk in range(4):
                                    mm(pB.t[:, :], wub.t[:, kk, ft * 128:(ft + 1) * 128], obT.t[:, kk, s_ * 512:(s_ + 1) * 512], kk == 0, kk == 3,
                                       r=[wub.k, obT.k], w=[pB.k])
                                act(gab[0].t[:], pGa.t[:, :], AF.Sigmoid, r=[pGa.k, b_ga.k], w=[gab[0].k], bias=b_ga.t[:, ft:ft + 1])
                                act(gab[1].t[:], pGb.t[:, :], AF.Sigmoid, r=[pGb.k, b_gb.k], w=[gab[1].k], bias=b_gb.t[:, ft:ft + 1])
                                tt("dve", t12[0].t[:], gab[0].t[:], pA.t[:, :], ALU.mult, r=[gab[0].k, pA.k], w=[t12[0].k])
                                tt("dve", t12[1].t[:], gab[1].t[:], pB.t[:, :], ALU.mult, r=[gab[1].k, pB.k], w=[t12[1].k])
                                tt("dve", mgT.t[:, ft, :], t12[0].t[:], t12[1].t[:], ALU.add, r=[t12[0].k, t12[1].k], w=[mgT.k])
                        if _stop(7):
                            return
                        wo = [ld(L["w_out"][:, hf * 512:(hf + 1) * 512].rearrange("(k p) n -> p k n", p=128)) for hf in range(2)]
                        def D1(c):
                            tq_ = s_ * 4 + c
                            row0 = b * SEQ + tq_ * 128
                            xb_ = xs2[c % 2]
                            h2f = h2f2[c % 2]
                            h2fk = h2fk2[c % 2]
                            dma("sp", xb_.t[:], x[b, tq_ * 128:(tq_ + 1) * 128, :], r=[], w=[xb_.k])
                            for hf in range(2):
                                for kk in range(8):
                                    mm(Q[hf].t[:, :], mgT.t[:, kk, c * 128:(c + 1) * 128], wo[hf].t[:, kk, :], kk == 0, kk == 7, r=[mgT.k, wo[hf].k], w=[Q[hf].k])
                                tt("dve", tD.t[:, hf * 512:(hf + 1) * 512], Q[hf].t[:, :], gmix.t[:, hf * 512:(hf + 1) * 512], ALU.mult,
                                   r=[Q[hf].k, gmix.k], w=[tD.k])
                            tt("dve", xb_.t[:], xb_.t[:], tD.t[:], ALU.add, r=[xb_.k, tD.k], w=[xb_.k])
                            dma("sp", x1s[row0:row0 + 128, :], xb_.t[:], r=[xb_.k], w=[T_x1s])
                            S.op("dve", lambda e: e.memset(ss2.t[:, 0:1], 0.0), w=[ss2.k])
                            act(junk2.t[:], xb_.t[:], AF.Square, r=[xb_.k], w=[junk2.k, ss2.k], accum=ss2.t[:, 0:1])
                            kb.rstd2(ss2.t[:, 1:2], ss2.t[:, 0:1], ss2.t[:, 2:3], [ss2.k], [ss2.k], 1.0 / D)
                            act(xn2.t[:], xb_.t[:], AF.Identity, r=[xb_.k, ss2.k], w=[xn2.k], scale=ss2.t[:, 2:3])
                            for kk in range(8):
                                pb = Q[2 + kk // 4]
                                tr(q4(pb)[:, kk % 4, :], xn2.t[:, kk * 128:(kk + 1) * 128], ident.t[:], r=[xn2.k, ident.k], w=[pb.k])
                            for kk in range(8):
                                pb = Q[2 + kk // 4]
                                if kk < 4:
                                    act(h2f.t[:, kk, :], q4(pb)[:, kk % 4, :], AF.Identity, r=[pb.k, A2.k, modT.k], w=[h2fk[0]],
                                        scale=A2.t[:, kk, b:b + 1], bias=modT.t[:, 24 + kk, b:b + 1])
                                else:
                                    stt(h2f.t[:, kk, :], q4(pb)[:, kk % 4, :], A2.t[:, kk, b:b + 1], modT.t[:, 24 + kk, b:b + 1].to_broadcast([128, 128]),
                                        ALU.mult, ALU.add, r=[pb.k, A2.k, modT.k], w=[h2fk[1]])
                            cp("act", h2b.t[:], h2f.t[:], r=h2fk, w=[h2b.k])
                            dma("sp", h2s[b, :, :, tq_ * 128:(tq_ + 1) * 128], h2b.t[:], r=[h2b.k], w=[T_h2s])

                        def D2(c):
                            tq_ = s_ * 4 + c
                            row0 = b * SEQ + tq_ * 128
                            h2f = h2f2[c % 2]
                            h2fk = h2fk2[c % 2]
                            rt = rt2[c % 2]
                            m8 = m82[c % 2]
                            for kk in range(8):
                                mm(Q[4].t[:, 0:NE], h2f.t[:, kk, :], wr.t[:, kk, :], kk == 0, kk == 7, r=h2fk + [wr.k], w=[Q[4].k])
                            act(rt.t[:, 0, :], Q[4].t[:, 0:NE], AF.Sigmoid, r=[Q[4].k], w=[rt.k])
                            tt("dve", rt.t[:, 1, :], rt.t[:, 0, :], brt_bc.t[:], ALU.add, r=[rt.k, brt_bc.k], w=[rt.k])
                            S.op("dve", lambda e: e.max(out=m8.t[:, 0:8], in_=rt.t[:, 1, :]), r=[rt.k], w=[m8.k])
                            ts("dve", rt.t[:, 2, :], rt.t[:, 1, :], m8.t[:, 7:8], ALU.is_ge, r=[rt.k, m8.k], w=[rt.k])
                            tt("dve", rt.t[:, 3, :], rt.t[:, 2, :], rt.t[:, 0, :], ALU.mult, r=[rt.k], w=[rt.k])
                            S.op("dve", lambda e: e.reduce_sum(out=m8.t[:, 8:9], in_=rt.t[:, 3, :], axis=AXX), r=[rt.k], w=[m8.k])
                            S.op("dve", lambda e: e.reciprocal(out=m8.t[:, 9:10], in_=m8.t[:, 8:9]), r=[m8.k], w=[m8.k])
                            ts("dve", rt.t[:, 4, :], rt.t[:, 3, :], m8.t[:, 9:10], ALU.mult, r=[rt.k, m8.k], w=[rt.k], s2=2.5, op1=ALU.mult)
                            dma("sp", Gs[row0:row0 + 128, :], rt.t[:, 4, :], r=[rt.k], w=[T_Gs])

                        D1(0)
                        for c in range(4):
                            if c + 1 < 4:
                                D1(c + 1)
                            D2(c)
                        if _stop(8) and s_ == KSS:
                            return
                S.barrier()
        S.barrier()


def _prep_common(inp):
    f = lambda a: np.ascontiguousarray(a, dtype=np.float32)
    fm = lambda v: f(v.reshape(-1, 128).T)
    w_in0 = inp["w_in"][0]
    b_in0 = inp["b_in"][0]
    t = np.arange(64)
    partner = (t // 32) * 32 + (1 - (t % 32) // 16) * 16 + (t % 16)
    TQ, TK, TV, NQ, NK, NV, GA, GB = 2048, 2112, 2176, 2240, 2752, 3264, 3776, 4800
    t4_cols = np.concatenate([TQ + t, TQ + partner, TK + t, TK + partner])
    w_in_ext = np.concatenate([w_in0, w_in0[:, t4_cols]], axis=1)
    b_t4 = b_in0[t4_cols].reshape(2, 128).T
    rpb = inp["na_rpb"][0]
    qr = np.arange(128) // 64
    qc = np.arange(128) % 64
    deltas = [-3, -2, -1, 0, 1, 2, 3, -2, 2]
    c_start = np.clip(qc - 8, 0, 48)
    nabs, masks = [], []
    for ti, dl in enumerate(deltas):
        dr = 2 * dl + qr[None, :] - qr[:, None]
        dc = qc[None, :] - qc[:, None]
        colok = (qc[None, :] >= c_start[:, None]) & (qc[None, :] < c_start[:, None] + 16)
        rowok = np.abs(dr) <= 7
        if ti >= 7:
            rowok = (dr >= -4) & (dr <= 3)
        ok = colok & rowok
        ai = np.clip(dr + 7, 0, 14)
        bi = np.clip(dc + 15, 0, 30)
        nabs.append(rpb[:, ai, bi])
        masks.append(np.where(ok, 0.0, NEG))
    nabs = np.stack(nabs, axis=1)
    nab2 = f(nabs.reshape(4, 4, 9, 128, 128).transpose(0, 2, 4, 1, 3))
    nmk2 = f(np.stack(masks, axis=0).transpose(0, 2, 1))
    tok = np.arange(SEQ)
    pos = np.stack([tok // 64, tok % 64], -1).astype(np.float32)
    inv_freq = (10000.0 ** (-np.arange(16, dtype=np.float32) / 16)).astype(np.float32)
    ang = pos[:, :, None] * inv_freq
    a_idx, h_idx, f_idx = t // 32, (t % 32) // 16, t % 16
    cosT = np.cos(ang)[:, a_idx, f_idx].T
    sinT = np.sin(ang)[:, a_idx, f_idx].T * np.where(h_idx == 0, -1.0, 1.0)[:, None]
    cs = f(np.concatenate([cosT, sinT], 0))
    hm = np.zeros((128, 4), np.float32)
    for hh in range(4):
        hm[32 * hh:32 * hh + 32, hh] = 1.0
    stacki = np.concatenate([np.eye(64), np.eye(64)], 0).astype(np.float32)
    epl = np.zeros((32, 4, 128), np.float32)
    for hh in range(4):
        epl[np.arange(32), hh, 32 * hh + np.arange(32)] = 1.0
    w_gu = np.concatenate([np.concatenate([inp["w_e_gate"][0], inp["w_e_up"][0]], axis=2),
                           np.concatenate([inp["w_sh_gate"][0], inp["w_sh_up"][0]], axis=1)[None]], axis=0)
    w_d = np.concatenate([inp["w_e_down"][0], inp["w_sh_down"][0][None]], axis=0)
    return dict(
        w_ada=f(inp["w_ada"][0]), b_ada=f(inp["b_ada"][0][None]), nmg=fm(inp["norm_mix_g"][0]), nfg=fm(inp["norm_ffn_g"][0]),
        w_in=f(w_in_ext), b_u=fm(b_in0[0:1024]), b_ga=fm(b_in0[GA:GB]), b_gb=fm(b_in0[GB:5824]),
        b_nq=fm(b_in0[NQ:NK]), b_nk=fm(b_in0[NK:NV]), b_nv=fm(b_in0[NV:GA]), b_t4=f(b_t4), b_tv=f(b_in0[TV:NQ][:, None]),
        b_v=f(b_in0[1024:2048][None]), ln_g=f(inp["sgu_ln_g"][0][None]), ln_b=f(inp["sgu_ln_b"][0][None]),
        w_s=f(inp["sgu_w_s"][0]), b_s=f(inp["sgu_b_s"][0].reshape(1, -1)), tiny_w_o=f(inp["tiny_w_o"][0]),
        nab2=nab2, nmk2=nmk2, cs=cs, hmask=hm, stacki=stacki, eplace=epl,
        w_up_a=f(inp["w_up_a"][0]), w_up_b=f(inp["w_up_b"][0]), w_out=f(inp["w_out"][0]),
        w_router=f(inp["w_router"][0]), b_router=f(inp["b_router"][0][None]),
        w_gu=f(w_gu), w_d=f(w_d), fng=f(inp["final_norm_g"][None]),
    )


def _core_inputs(inp, common, core, NB):
    b0 = core * NB
    cc = np.concatenate([inp["c"][b0:b0 + NB], inp["c_ctx"][None]], 0)
    if NB < 4:
        cc = np.concatenate([cc[:NB], np.zeros((4 - NB, D), np.float32), cc[NB:]], 0)
    cT = np.ascontiguousarray(cc.T.reshape(8, 128, 5).transpose(1, 0, 2), dtype=np.float32)
    m = dict(common)
    m.update(x=np.ascontiguousarray(inp["x"][b0:b0 + NB]), ctx=np.ascontiguousarray(inp["ctx"][b0:b0 + NB]), cT=cT)
    return m


_NC_CACHE = {}


def kernel(**inputs):
    inp = {k: np.asarray(v) for k, v in inputs.items()}
    NB = 4
    if "nc" not in _NC_CACHE:
        _NC_CACHE["nc"] = build(NB)
    nc = _NC_CACHE["nc"]
    common = _prep_common(inp)
    in_maps = [_core_inputs(inp, common, c, NB) for c in range(8)]
    res = run_bass_kernel_spmd(nc, in_maps, core_ids=list(range(8)))
    return np.concatenate([r["y"] for r in res.results], axis=0).astype(np.float32)
```

```python
import numpy as np
from contextlib import ExitStack
import concourse.bass as bass
import concourse.mybir as mybir
from concourse.bass_utils import run_bass_kernel_spmd

F32 = mybir.dt.float32
BF16 = mybir.dt.bfloat16
AF = mybir.ActivationFunctionType
ALU = mybir.AluOpType

import os as _os0
USE_POW = int(_os0.environ.get("KPOW", "1"))
SEM_LIMIT = 20000
N_DMA_SEMS = 40

D = 1024
SEQ = 2048
CTX = 256
NKEY = SEQ + CTX
NE = 64
EPS = 1e-6
NA_SCALE = 32 ** -0.5
TINY_SCALE = 64 ** -0.5
NEG = -30000.0


class Sched:
    ENGS = ("pe", "act", "dve", "pool", "sp")

    def __init__(self, nc, es, same_engine_sync=("pool",), same_engine_raw=("act", "dve", "pool")):
        self.nc = nc
        self.es = es
        self.ins = []
        self.state = {}
        self.same = set(same_engine_sync)
        self.same_raw = set(same_engine_raw)
        self.bar = {e: set() for e in self.ENGS}
        self.last = {}
        self.dmas_since_bar = []
        self.dma_hist = []
        self._tok = 0

    def tok(self):
        self._tok += 1
        return self._tok

    def op(self, eng, fn, r=(), w=(), dma=False):
        idx = len(self.ins)
        key = ("dma", idx) if dma else eng
        deps = set()
        force = dma or (eng in self.same)
        force_raw = force or (eng in self.same_raw)
        for t in r:
            st = self.state.setdefault(t, [{}, {}])
            for k, i in st[0].items():
                if k != key or force_raw:
                    deps.add(i)
        for t in w:
            st = self.state.setdefault(t, [{}, {}])
            for k, i in st[0].items():
                if k != key or force:
                    deps.add(i)
            for k, i in st[1].items():
                if k != key or force:
                    deps.add(i)
        for t in r:
            self.state[t][1][key] = idx
        for t in w:
            self.state[t] = [{key: idx}, {}]
        if self.bar[eng]:
            deps |= self.bar[eng]
            self.bar[eng] = set()
        if dma:
            if len(self.dma_hist) >= N_DMA_SEMS:
                deps.add(self.dma_hist[-N_DMA_SEMS])
            self.dma_hist.append(idx)
            self.dmas_since_bar.append(idx)
        deps.discard(idx)
        self.ins.append(dict(eng=eng, fn=fn, deps=deps, dma=dma))
        self.last[eng] = idx
        return idx

    def barrier(self):
        s = set(self.last.values()) | set(self.dmas_since_bar)
        for e in self.ENGS:
            self.bar[e] |= s
        self.dmas_since_bar = []

    def emit(self):
        nc, es = self.nc, self.es
        needed = set()
        for ins in self.ins:
            needed |= ins["deps"]
        eng_sems = {e: [] for e in self.ENGS}
        eng_cnt = {e: SEM_LIMIT for e in self.ENGS}
        dma_sems = [es.enter_context(nc.semaphore(f"dq{i}")) for i in range(N_DMA_SEMS)]
        nd = 0
        for i, ins in enumerate(self.ins):
            if ins["dma"]:
                ins["sem"] = ("d", nd % N_DMA_SEMS)
                ins["val"] = 16 * (nd // N_DMA_SEMS + 1)
                ins["semh"] = dma_sems[nd % N_DMA_SEMS]
                nd += 1
            elif i in needed:
                e = ins["eng"]
                if eng_cnt[e] >= SEM_LIMIT:
                    eng_sems[e].append(es.enter_context(nc.semaphore(f"s_{e}{len(eng_sems[e])}")))
                    eng_cnt[e] = 0
                eng_cnt[e] += 1
                ins["sem"] = (e, len(eng_sems[e]) - 1)
                ins["val"] = eng_cnt[e]
                ins["semh"] = eng_sems[e][-1]
        progs = {e: [] for e in self.ENGS}
        waited = {e: {} for e in self.ENGS}
        for i, ins in enumerate(self.ins):
            e = ins["eng"]
            waits = {}
            for d in ins["deps"]:
                p = self.ins[d]
                if "sem" not in p:
                    continue
                sk = p["sem"]
                if sk[0] != "d":
                    cur = waited[e].get(("E", sk[0]), (-1, 0))
                    if (sk[1], p["val"]) <= cur:
                        continue
                    prev = waits.get(("E", sk[0]))
                    if prev is None or (sk[1], p["val"]) > (prev[0], prev[1]):
                        waits[("E", sk[0])] = (sk[1], p["val"], p["semh"])
                else:
                    cur = waited[e].get(sk, 0)
                    if p["val"] <= cur:
                        continue
                    prev = waits.get(sk)
                    if prev is None or p["val"] > prev[1]:
                        waits[sk] = (0, p["val"], p["semh"])
            wl = []
            for k, (ep, v, h) in waits.items():
                waited[e][k] = (ep, v) if k[0] == "E" else v
                wl.append((h, v))
            progs[e].append((wl, ins["fn"], ins.get("semh"), 16 if ins["dma"] else 1))
        self.stats = {e: len(progs[e]) for e in self.ENGS}

        def run(eng, prog):
            for wl, fn, semh, n in prog:
                for h, v in wl:
                    eng.wait_ge(h, v)
                if fn is None:
                    continue
                r = fn(eng)
                if semh is not None:
                    r.then_inc(semh, n)

        block = es.enter_context(nc.Block())

        @block.tensor
        def _(eng):
            run(eng, progs["pe"])

        @block.scalar
        def _(eng):
            run(eng, progs["act"])

        @block.vector
        def _(eng):
            run(eng, progs["dve"])

        @block.gpsimd
        def _(eng):
            run(eng, progs["pool"])

        @block.sync
        def _(eng):
            run(eng, progs["sp"])


class B:
    def __init__(self, S, t):
        self.t = t
        self.k = S.tok()


class K:
    def __init__(self, nc, S):
        self.nc, self.S = nc, S

    def _nm(self, name):
        self._n = getattr(self, "_n", 0) + 1
        return f"{name}_{self._n}"

    def sb(self, es, name, shape, dt):
        return B(self.S, es.enter_context(self.nc.sbuf_tensor(self._nm(name), shape, dt)))

    def ps(self, es, name, shape, dt=F32):
        return B(self.S, es.enter_context(self.nc.psum_tensor(self._nm(name), shape, dt)))

    def mm(self, out, lhsT, rhs, start, stop, r, w):
        self.S.op("pe", lambda e: e.matmul(out, lhsT=lhsT, rhs=rhs, start=start, stop=stop), r=r, w=w)

    def tr(self, out, in_, ident, r, w):
        self.S.op("pe", lambda e: e.transpose(out=out, in_=in_, identity=ident), r=r, w=w)

    def act(self, out, in_, func, r, w, bias=None, scale=None, accum=None):
        kw = {}
        if bias is not None:
            kw["bias"] = bias
        if scale is not None:
            kw["scale"] = scale
        if accum is not None:
            kw["accum_out"] = accum
        self.S.op("act", lambda e: e.activation(out=out, in_=in_, func=func, **kw), r=r, w=w)

    def tt(self, eng, out, in0, in1, op, r, w):
        self.S.op(eng, lambda e: e.tensor_tensor(out=out, in0=in0, in1=in1, op=op), r=r, w=w)

    def ts(self, eng, out, in0, s1, op0, r, w, s2=None, op1=None):
        if op1 is None:
            self.S.op(eng, lambda e: e.tensor_scalar(out=out, in0=in0, scalar1=s1, scalar2=None, op0=op0), r=r, w=w)
        else:
            self.S.op(eng, lambda e: e.tensor_scalar(out=out, in0=in0, scalar1=s1, scalar2=s2, op0=op0, op1=op1), r=r, w=w)

    def stt(self, out, in0, scalar, in1, op0, op1, r, w):
        self.S.op("dve", lambda e: e.scalar_tensor_tensor(out=out, in0=in0, scalar=scalar, in1=in1, op0=op0, op1=op1), r=r, w=w)

    def cp(self, eng, out, in_, r, w):
        if eng == "act":
            self.S.op("act", lambda e: e.copy(out=out, in_=in_), r=r, w=w)
        else:
            self.S.op(eng, lambda e: e.tensor_copy(out=out, in_=in_), r=r, w=w)

    def dma(self, eng, out, in_, r, w):
        self.S.op(eng, lambda e: e.dma_start(out=out, in_=in_), r=r, w=w, dma=True)

    def rstd2(self, tmp, ss, out, rk, wk, scale):
        if USE_POW:
            self.S.op("dve", lambda e: e.tensor_scalar(out=tmp, in0=ss, scalar1=scale, scalar2=EPS, op0=ALU.mult, op1=ALU.add), r=rk, w=wk)
            self.S.op("pool", lambda e: e.tensor_tensor(out=out, in0=tmp, in1=self.mhalf.t[:, 0:1], op=ALU.pow), r=wk + [self.mhalf.k], w=wk)
        else:
            self.act(tmp, ss, AF.Sqrt, r=rk, w=wk, bias=self.epsb.t[:, 0:1], scale=scale)
            self.S.op("dve", lambda e: e.reciprocal(out=out, in_=tmp), r=wk, w=wk)

    def rstd(self, es_bufs, ss, out, r_k, scale):
        tmp = es_bufs
        self.act(tmp.t[:, 0:1], ss, AF.Sqrt, r=r_k, w=[tmp.k], bias=self.epsb.t[:, 0:1], scale=scale)
        self.S.op("dve", lambda e: e.reciprocal(out=out[0], in_=tmp.t[:, 0:1]), r=[tmp.k], w=[out[1]])


def build(NB=4, debug=None):
    nc = bass.Bass("TRN2", target_bir_lowering=False)
    NT = NB * SEQ

    def din(name, shape, dt=F32):
        return nc.dram_tensor(name, list(shape), dt, kind="ExternalInput").ap()

    x = din("x", [NB, SEQ, D])
    ctx = din("ctx", [NB, CTX, D])
    cT = din("cT", [128, 8, 5])
    w_ada = din("w_ada", [D, 6 * D])
    b_ada = din("b_ada", [1, 6 * D])
    nmg = din("nmg", [128, 8])
    nfg = din("nfg", [128, 8])
    w_in = din("w_in", [D, 5824 + 256])
    b_u = din("b_u", [128, 8])
    b_ga = din("b_ga", [128, 8])
    b_gb = din("b_gb", [128, 8])
    b_nq = din("b_nq", [128, 4])
    b_nk = din("b_nk", [128, 4])
    b_nv = din("b_nv", [128, 4])
    b_t4 = din("b_t4", [128, 2])
    b_tv = din("b_tv", [64, 1])
    b_v = din("b_v", [1, D])
    ln_g = din("ln_g", [1, D])
    ln_b = din("ln_b", [1, D])
    w_s = din("w_s", [8, 128, 128])
    b_s = din("b_s", [1, D])
    tiny_w_o = din("tiny_w_o", [64, D])
    nab2 = din("nab2", [4, 9, 128, 4, 128])
    nmk2 = din("nmk2", [9, 128, 128])
    cs = din("cs", [128, SEQ])
    hmask = din("hmask", [128, 4])
    stacki = din("stacki", [128, 64])
    eplace = din("eplace", [32, 4, 128])
    w_up_a = din("w_up_a", [D, D])
    w_up_b = din("w_up_b", [512, D])
    w_out = din("w_out", [D, D])
    w_router = din("w_router", [D, NE])
    b_router = din("b_router", [1, NE])
    w_gu = din("w_gu", [NE + 1, D, 512])
    w_d = din("w_d", [NE + 1, 256, D])
    fng = din("fng", [1, D])
    y = nc.dram_tensor("y", [NB, SEQ, D], F32, kind="ExternalOutput").ap()

    kM = "ExternalOutput" if debug == "M" else ("ExternalInput" if debug == "E" else "Internal")
    x1s = nc.dram_tensor("x1s", [NT, D], F32, kind=kM).ap()
    h2s = nc.dram_tensor("h2s", [NB, 128, 8, SEQ], BF16, kind=kM).ap()
    Gs = nc.dram_tensor("Gs", [NT, NE], F32, kind=kM).ap()
    gsc = nc.dram_tensor("gsc", [NB, 2, D], F32, kind=kM).ap()
    hTs = nc.dram_tensor("hTs", [NB, 4, 128, 8, 512], BF16, kind="Internal").ap()
    EBs = nc.dram_tensor("EBs", [128, 4, 9, 512], BF16, kind="Internal").ap()

    es = ExitStack()
    with es:
        S = Sched(nc, es)
        kb = K(nc, S)
        sb, ps, mm, tr, act, tt, ts, stt, cp, dma = kb.sb, kb.ps, kb.mm, kb.tr, kb.act, kb.tt, kb.ts, kb.stt, kb.cp, kb.dma
        T_y = S.tok()
        T_x1s, T_h2s, T_Gs, T_gsc, T_hTs = S.tok(), S.tok(), S.tok(), S.tok(), S.tok()
        T_EBs = S.tok()

        ident = sb(es, "ident", [128, 128], F32)
        identb = sb(es, "identb", [128, 128], BF16)
        epsb = sb(es, "epsb", [128, 1], F32)
        kb.epsb = epsb
        S.op("pool", lambda e: e.memset(ident.t[:], 0.0), w=[ident.k])
        S.op("pool", lambda e: e.affine_select(out=ident.t[:], in_=ident.t[:], pattern=[[-1, 128]], compare_op=ALU.not_equal,
                                               fill=1.0, base=0, channel_multiplier=1), r=[ident.k], w=[ident.k])
        cp("dve", identb.t[:], ident.t[:], r=[ident.k], w=[identb.k])
        S.op("dve", lambda e: e.memset(epsb.t[:], EPS), w=[epsb.k])
        mhalf = sb(es, "mhalf", [128, 1], F32)
        kb.mhalf = mhalf
        S.op("pool", lambda e: e.memset(mhalf.t[:], -0.5), w=[mhalf.k])
        fng_bc = sb(es, "fng_bc", [128, D], F32)
        dma("sp", fng_bc.t[:], fng.to_broadcast([128, D]), r=[], w=[fng_bc.k])

        if debug != "E":
            phase_m(nc, S, kb, es, locals())
        S.barrier()
        if debug != "M":
            phase_e(nc, S, kb, es, locals())
        S.op("sp", None, r=[T_y, T_x1s, T_h2s, T_Gs, T_gsc])
        S.emit()
    return nc


def phase_e(nc, S, kb, es0, L):
    sb, ps, mm, tr, act, tt, ts, stt, cp, dma = kb.sb, kb.ps, kb.mm, kb.tr, kb.act, kb.tt, kb.ts, kb.stt, kb.cp, kb.dma
    NB = L["NB"]
    x1s, h2s, Gs, gsc, y, w_gu, w_d = L["x1s"], L["h2s"], L["Gs"], L["gsc"], L["y"], L["w_gu"], L["w_d"]
    T_y, T_x1s, T_h2s, T_Gs, T_gsc = L["T_y"], L["T_x1s"], L["T_h2s"], L["T_Gs"], L["T_gsc"]
    identb, fng_bc = L["identb"], L["fng_bc"]
    es = ExitStack()
    with es:
        acc = sb(es, "acc", [128, 16, D], F32)
        acck = [S.tok() for _ in range(16)]
        h2T = sb(es, "h2T", [128, 8, SEQ], BF16)
        Gt = [sb(es, f"Gt{i}", [128, 16, NE], F32) for i in range(2)]
        gffn = [sb(es, f"gffn{i}", [128, D], F32) for i in range(2)]
        wgu = [sb(es, f"wgu{i}", [128, 8, 512], BF16) for i in range(4)]
        wd = [sb(es, f"wd{i}", [128, 2, D], BF16) for i in range(4)]
        sg = [sb(es, f"sg{i}", [128, 256], F32) for i in range(2)]
        a_ = [sb(es, f"a{i}", [128, 256], BF16) for i in range(2)]
        aT = [sb(es, f"aT{i}", [128, 2, 128], BF16) for i in range(2)]
        x1t = [sb(es, f"x1t{i}", [128, D], F32) for i in range(4)]
        ssk = [S.tok() for _ in range(4)]
        ot = [sb(es, f"ot{i}", [128, D], F32) for i in range(2)]
        junk = sb(es, "junkE", [128, D], BF16)
        ssE = sb(es, "ssE", [128, 8], F32)
        pY = [ps(es, f"pY{i}", [128, 2, 512]) for i in range(2)]
        pGU = [ps(es, f"pGU{i}", [128, 512]) for i in range(2)]
        pT = [ps(es, f"pT{i}", [128, 2, 128], BF16) for i in range(2)]

        pairs = [(2 * i, 2 * i + 1) for i in range(NE // 2)] + [(NE,)]
        NP = len(pairs)
        wslots = {}
        wcount = [0]

        def issue_loads(k):
            if k >= NB * NP or k in wslots:
                return
            pair = pairs[k % NP]
            wsl = {}
            for e in pair:
                sl = wcount[0] % 4
                wcount[0] += 1
                wsl[e] = sl
                dma("pool", wgu[sl].t[:], w_gu[e].rearrange("(k p) n -> p k n", p=128), r=[], w=[wgu[sl].k])
                dma("pool", wd[sl].t[:], w_d[e].rearrange("(j p) n -> p j n", p=128), r=[], w=[wd[sl].k])
            wslots[k] = wsl

        def load_h2T(b):
            dma("sp", h2T.t[:], h2s[b], r=[T_h2s], w=[h2T.k])

        def load_gates(b):
            dma("sp", Gt[b % 2].t[:], Gs[b * SEQ:(b + 1) * SEQ, :].rearrange("(t p) e -> p t e", p=128), r=[T_Gs], w=[Gt[b % 2].k])
            dma("sp", gffn[b % 2].t[:], gsc[b, 1:2, :].to_broadcast([128, D]), r=[T_gsc], w=[gffn[b % 2].k])

        def final_stage(b, t, st, tail=False):
            s4 = t % 4
            row0 = b * SEQ + t * 128
            gf = gffn[b % 2]
            c = 2 * s4
            xt = x1t[s4]
            if st == 0:
                dma("sp", xt.t[:], x1s[row0:row0 + 128, :], r=[T_x1s], w=[xt.k])
                tt("dve" if tail else "pool", acc.t[:, t, :], acc.t[:, t, :], gf.t[:], ALU.mult, r=[acck[t], gf.k], w=[acck[t]])
                tt("dve", xt.t[:], xt.t[:], acc.t[:, t, :], ALU.add, r=[xt.k, acck[t]], w=[xt.k])
            elif st == 1:
                S.op("dve", lambda e: e.memset(ssE.t[:, c:c + 1], 0.0), w=[ssk[s4]])
                act(junk.t[:], xt.t[:], AF.Square, r=[xt.k], w=[junk.k, ssk[s4]], accum=ssE.t[:, c:c + 1])
            elif st == 2:
                kb.rstd2(ssE.t[:, c + 1:c + 2], ssE.t[:, c:c + 1], ssE.t[:, c:c + 1], [ssk[s4]], [ssk[s4]], 1.0 / D)
            else:
                o_ = ot[t % 2]
                stt(o_.t[:], xt.t[:], ssE.t[:, c:c + 1], fng_bc.t[:], ALU.mult, ALU.mult,
                    r=[xt.k, ssk[s4], fng_bc.k], w=[o_.k])
                dma("sp", y[b, t * 128:(t + 1) * 128, :], o_.t[:], r=[o_.k], w=[T_y])

        def final_tile(b, t):
            for st in range(4):
                final_stage(b, t, st, tail=True)

        load_h2T(0)
        load_gates(0)
        issue_loads(0)
        for b in range(NB):
            Gb = Gt[b % 2]
            for pi, pair in enumerate(pairs):
                kglob = b * NP + pi
                issue_loads(kglob + 1)
                wsl = wslots[kglob]
                if pi == 1 and b + 1 < NB:
                    load_gates(b + 1)
                items = [(t, e) for t in range(16) for e in pair]
                n = len(items)

                def GU(i):
                    t, e = items[i]
                    s = i % 2
                    for k in range(8):
                        mm(pGU[s].t[:, :], h2T.t[:, k, t * 128:(t + 1) * 128], wgu[wsl[e]].t[:, k, :], k == 0, k == 7,
                           r=[h2T.k, wgu[wsl[e]].k], w=[pGU[s].k])

                def EW(i):
                    t, e = items[i]
                    s = i % 2
                    act(sg[s].t[:], pGU[s].t[:, 0:256], AF.Silu, r=[pGU[s].k], w=[sg[s].k])
                    gsc_ = 1.0 if e == NE else Gb.t[:, t, e:e + 1]
                    stt(a_[s].t[:], sg[s].t[:], gsc_, pGU[s].t[:, 256:512], ALU.mult, ALU.mult,
                        r=[sg[s].k, Gb.k, pGU[s].k], w=[a_[s].k])

                def TR(i):
                    s = i % 2
                    for j in range(2):
                        tr(pT[s].t[:, j, :], a_[s].t[:, j * 128:(j + 1) * 128], identb.t[:], r=[a_[s].k, identb.k], w=[pT[s].k])
                    cp("act", aT[s].t[:], pT[s].t[:], r=[pT[s].k], w=[aT[s].k])

                def DN(i):
                    t, e = items[i]
                    s = i % 2
                    ys = t % 2
                    first = (e == pair[0])
                    last = (e == pair[-1])
                    for nh in range(2):
                        for j in range(2):
                            mm(pY[ys].t[:, nh, :], aT[s].t[:, j, :], wd[wsl[e]].t[:, j, nh * 512:(nh + 1) * 512],
                               first and j == 0, last and j == 1, r=[aT[s].k, wd[wsl[e]].k], w=[pY[ys].k])
                    if last:
                        if pi == 0:
                            cp("dve", acc.t[:, t, :], pY[ys].t[:].rearrange("p a b -> p (a b)"), r=[pY[ys].k], w=[acck[t]])
                        else:
                            tt("dve", acc.t[:, t, :], acc.t[:, t, :], pY[ys].t[:].rearrange("p a b -> p (a b)"), ALU.add,
                               r=[pY[ys].k, acck[t]], w=[acck[t]])

                GU(0)
                for i in range(n):
                    if b > 0 and pi < 2:
                        g_ = pi * 32 + i
                        for st in (3, 2, 1, 0):
                            tq2 = g_ - st
                            if tq2 >= 0 and tq2 % 2 == 0 and tq2 // 2 < 16:
                                final_stage(b - 1, tq2 // 2, st)
                    if i + 1 < n:
                        GU(i + 1)
                    EW(i)
                    TR(i)
                    if i >= 1:
                        DN(i - 1)
                DN(n - 1)
            if b + 1 < NB:
                load_h2T(b + 1)
        for g_ in range(36):
            for st in (3, 2, 1, 0):
                tq2 = g_ - st
                if tq2 >= 0 and tq2 % 2 == 0 and tq2 // 2 < 16:
                    final_stage(NB - 1, tq2 // 2, st, tail=True)
        S.barrier()


class _Stop(Exception):
    pass


import os as _os
STOP = int(_os.environ.get("KSTOP", "0"))
EVAC_ACT_ONLY = int(_os.environ.get("KEVAC", "0"))
NOLD = int(_os.environ.get("KNOLD", "0"))
NWST = int(_os.environ.get("KNWST", "5"))
KSS = int(_os.environ.get("KSS", "0"))
NAD = int(_os.environ.get("KNAD", "2"))


def phase_m(nc, S, kb, es0, L):
    _phase_m(nc, S, kb, es0, L)
    S.barrier()


def _stop(n):
    return STOP == n


def _phase_m(nc, S, kb, es0, L):
    sb, ps, mm, tr, act, tt, ts, stt, cp, dma = kb.sb, kb.ps, kb.mm, kb.tr, kb.act, kb.tt, kb.ts, kb.stt, kb.cp, kb.dma
    NB = L["NB"]
    x, ctx, w_in = L["x"], L["ctx"], L["w_in"]
    x1s, h2s, Gs, gsc, hTs = L["x1s"], L["h2s"], L["Gs"], L["gsc"], L["hTs"]
    T_x1s, T_h2s, T_Gs, T_gsc, T_hTs = L["T_x1s"], L["T_h2s"], L["T_Gs"], L["T_gsc"], L["T_hTs"]
    ident, identb, epsb = L["ident"], L["identb"], L["epsb"]
    AXX = mybir.AxisListType.X
    esM = ExitStack()
    with esM:
        def load(es, name, shape, dt, src, eng="sp"):
            b_ = sb(es, name, shape, dt)
            dma(eng, b_.t[:], src, r=[], w=[b_.k])
            return b_

        def kcols(a, b_):
            return w_in[:, a:b_].rearrange("(k p) n -> p k n", p=128)

        nmg_t = load(esM, "nmg_t", [128, 8], F32, L["nmg"])
        nfg_t = load(esM, "nfg_t", [128, 8], F32, L["nfg"])
        b_u = load(esM, "b_u_t", [128, 8], F32, L["b_u"])
        b_ga = load(esM, "b_ga_t", [128, 8], F32, L["b_ga"])
        b_gb = load(esM, "b_gb_t", [128, 8], F32, L["b_gb"])
        b_nq = load(esM, "b_nq_t", [128, 4], F32, L["b_nq"])
        b_nk = load(esM, "b_nk_t", [128, 4], F32, L["b_nk"])
        b_nv = load(esM, "b_nv_t", [128, 4], F32, L["b_nv"])
        b_t4 = load(esM, "b_t4_t", [128, 2], F32, L["b_t4"])
        b_tv = load(esM, "b_tv_t", [64, 1], F32, L["b_tv"])
        bv_bc = load(esM, "bv_bc", [128, D], F32, L["b_v"].to_broadcast([128, D]))
        lng_bc = load(esM, "lng_bc", [128, D], F32, L["ln_g"].to_broadcast([128, D]))
        brt_bc = load(esM, "brt_bc", [128, NE], F32, L["b_router"].to_broadcast([128, NE]))
        CS = load(esM, "CS", [128, SEQ], F32, L["cs"])
        hmask = load(esM, "hmask_t", [128, 4], F32, L["hmask"])
        stacki = load(esM, "stacki_t", [128, 64], F32, L["stacki"])
        wr = load(esM, "wr", [128, 8, NE], F32, L["w_router"].rearrange("(k p) n -> p k n", p=128))
        two = load(esM, "two", [64, D], BF16, L["tiny_w_o"], eng="pool")
        epl = load(esM, "epl", [32, 4, 128], BF16, L["eplace"], eng="pool")
        ones_b = sb(esM, "ones_b", [128, 64], BF16)
        S.op("dve", lambda e: e.memset(ones_b.t[:], 1.0), w=[ones_b.k])
        modT = sb(esM, "modT", [128, 48, 5], F32)
        A1 = sb(esM, "A1", [128, 8, 5], F32)
        A2 = sb(esM, "A2", [128, 8, 5], F32)
        w_sT = sb(esM, "w_sT", [128, 8, 128], BF16)
        LB = sb(esM, "LB", [2, D], F32)
        RB = sb(esM, "RB", [2, D], F32)

        with ExitStack() as e1:
            scT = load(e1, "scT", [128, 8, 5], F32, L["cT"])
            act(scT.t[:], scT.t[:], AF.Silu, r=[scT.k], w=[scT.k])
            modrow = sb(e1, "modrow", [5, 6 * D], F32)
            bada = load(e1, "bada", [5, 6 * D], F32, L["b_ada"].to_broadcast([5, 6 * D]))
            wa = [sb(e1, f"wa{i}", [128, 8, 512], F32) for i in range(2)]
            pm = ps(e1, "pm", [128, 512])
            pmt = ps(e1, "pmt", [128, 48, 5])
            for cb in range(12):
                s_ = cb % 2
                dma("sp", wa[s_].t[:], L["w_ada"][:, cb * 512:(cb + 1) * 512].rearrange("(k p) n -> p k n", p=128), r=[], w=[wa[s_].k])
                for k in range(8):
                    mm(pm.t[0:5, :], scT.t[:, k, :], wa[s_].t[:, k, :], k == 0, k == 7, r=[scT.k, wa[s_].k], w=[pm.k])
                tt("dve", modrow.t[:, cb * 512:(cb + 1) * 512], pm.t[0:5, :], bada.t[:, cb * 512:(cb + 1) * 512], ALU.add,
                   r=[pm.k, bada.k], w=[modrow.k])
            for t in range(48):
                tr(pmt.t[:, t, :], modrow.t[0:5, t * 128:(t + 1) * 128], ident.t[0:5, 0:5], r=[modrow.k, ident.k], w=[pmt.k])
            cp("dve", modT.t[:], pmt.t[:], r=[pmt.k], w=[modT.k])
            for j in range(5):
                stt(A1.t[:, :, j], modT.t[:, 8:16, j], 1.0, nmg_t.t[:], ALU.add, ALU.mult, r=[modT.k, nmg_t.k], w=[A1.k])
                stt(A2.t[:, :, j], modT.t[:, 32:40, j], 1.0, nfg_t.t[:], ALU.add, ALU.mult, r=[modT.k, nfg_t.k], w=[A2.k])
            for b in range(NB):
                dma("sp", gsc[b, 0:1, :], modrow.t[b:b + 1, 2 * D:3 * D], r=[modrow.k], w=[T_gsc])
                dma("sp", gsc[b, 1:2, :], modrow.t[b:b + 1, 5 * D:6 * D], r=[modrow.k], w=[T_gsc])
            if _stop(1):
                return
            wsn = load(e1, "wsn", [128, 8, 128], F32, L["w_s"].rearrange("g p q -> p g q"))
            pw = [ps(e1, f"pw{i}", [128, 4, 128]) for i in range(2)]
            for g in range(8):
                tr(pw[g // 4].t[:, g % 4, :], wsn.t[:, g, :], ident.t[:], r=[wsn.k, ident.k], w=[pw[g // 4].k])
            for i in range(2):
                cp("dve", w_sT.t[:, 4 * i:4 * i + 4, :], pw[i].t[:], r=[pw[i].k], w=[w_sT.k])
            prs = ps(e1, "prs", [128, 2, 512])
            for g in range(8):
                mm(prs.t[0:1, g // 4, (g % 4) * 128:(g % 4 + 1) * 128], ones_b.t[:, 0:1], w_sT.t[:, g, :], True, True,
                   r=[ones_b.k, w_sT.k], w=[prs.k])
            cp("dve", RB.t[0:1, :], prs.t[0:1, :, :].rearrange("p a b -> p (a b)"), r=[prs.k], w=[RB.k])
            dma("sp", RB.t[1:2, :], L["b_s"], r=[], w=[RB.k])
            S.op("dve", lambda e: e.memset(LB.t[:], 1.0), w=[LB.k])
            dma("sp", LB.t[0:1, :], L["ln_b"], r=[], w=[LB.k])
        S.barrier()
        if _stop(2):
            return

        for b in range(NB):
            with ExitStack() as eb:
                tkT = sb(eb, "tkT", [64, NKEY], BF16)
                tva = sb(eb, "tva", [128, 18, 65], BF16)
                S.op("dve", lambda e: e.memset(tva.t[:, :, 64:65], 1.0), w=[tva.k])
                tqT = sb(eb, "tqT", [64, SEQ], BF16)
                obT = sb(eb, "obT", [128, 4, SEQ], BF16)
                gmix = sb(eb, "gmix", [128, D], F32)
                dma("sp", gmix.t[:], gsc[b, 0:1, :].to_broadcast([128, D]), r=[T_gsc], w=[gmix.k])
                with ExitStack() as e1:
                    nkT = sb(e1, "nkT", [128, 4, NKEY], BF16)
                    nva = sb(e1, "nva", [128, 18, 16, 33], BF16)
                    S.op("dve", lambda e: e.memset(nva.t[:, :, :, 32:33], 1.0), w=[nva.k])
                    nqT = sb(e1, "nqT", [128, 4, SEQ], BF16)
                    with ExitStack() as e1a:
                        wk1 = sb(e1a, "wk1", [128, 8, 1856], BF16)
                        for q_ in range(3):
                            dma("pool", wk1.t[:, :, q_ * 512:(q_ + 1) * 512], kcols(2240 + q_ * 512, 2240 + (q_ + 1) * 512), r=[], w=[wk1.k])
                        dma("pool", wk1.t[:, :, 1536:1792], kcols(5824, 6080), r=[], w=[wk1.k])
                        dma("pool", wk1.t[:, :, 1792:1856], kcols(2176, 2240), r=[], w=[wk1.k])
                        xs = [sb(e1a, f"xs{i}", [128, D], F32) for i in range(4)]
                        xn = [sb(e1a, f"xn{i}", [128, D], F32) for i in range(4)]
                        junk = sb(e1a, "junk1", [128, D], BF16)
                        ss = sb(e1a, "ss1", [128, 8], F32)
                        ssk1 = [S.tok() for _ in range(4)]
                        hT = [sb(e1a, f"hT{i}", [128, 8, 512], BF16) for i in range(2)]
                        hk = [[S.tok(), S.tok()] for _ in range(2)]
                        rbuf = sb(e1a, "rbuf", [128, 512], F32)
                        pX = [ps(e1a, f"pX{i}", [128, 4, 128]) for i in range(4)]
                        pP = [ps(e1a, f"pP{i}", [128, 512]) for i in range(3)]
                        p64 = ps(e1a, "p64", [128, 512])
                        pcnt = 0
                        tile_id = {}
                        for u_ in range(5):
                            for tl_ in range(4 if u_ < 4 else 2):
                                tile_id[(u_, tl_)] = len(tile_id)

                        def T1(u):
                            for tl in range(4 if u < 4 else 2):
                                g_ = tile_id[(u, tl)]
                                s_ = g_ % 4
                                src = x[b, u * 512 + tl * 128:u * 512 + (tl + 1) * 128, :] if u < 4 else ctx[b, tl * 128:(tl + 1) * 128, :]
                                dma("sp", xs[s_].t[:], src, r=[], w=[xs[s_].k])
                                c = 2 * s_
                                S.op("dve", lambda e, c=c: e.memset(ss.t[:, c:c + 1], 0.0), w=[ssk1[s_]])
                                act(junk.t[:], xs[s_].t[:], AF.Square, r=[xs[s_].k], w=[junk.k, ssk1[s_]], accum=ss.t[:, c:c + 1])
                                kb.rstd2(ss.t[:, c + 1:c + 2], ss.t[:, c:c + 1], ss.t[:, c:c + 1], [ssk1[s_]], [ssk1[s_]], 1.0 / D)
                                act(xn[s_].t[:], xs[s_].t[:], AF.Identity, r=[xs[s_].k, ssk1[s_]], w=[xn[s_].k], scale=ss.t[:, c:c + 1])

                        def T2(u):
                            j = b if u < 4 else 4
                            h = hT[u % 2]
                            hkk = hk[u % 2]
                            for tl in range(4 if u < 4 else 2):
                                g_ = tile_id[(u, tl)]
                                s_ = g_ % 4
                                p2 = g_ % 2
                                for kk in range(8):
                                    pb = pX[2 * p2 + kk // 4]
                                    tr(pb.t[:, kk % 4, :], xn[s_].t[:, kk * 128:(kk + 1) * 128], ident.t[:], r=[xn[s_].k, ident.k], w=[pb.k])
                                for kk in range(8):
                                    pb = pX[2 * p2 + kk // 4]
                                    o_ = h.t[:, kk, tl * 128:(tl + 1) * 128]
                                    if kk < 4:
                                        act(o_, pb.t[:, kk % 4, :], AF.Identity, r=[pb.k, A1.k, modT.k], w=[hkk[0]],
                                            scale=A1.t[:, kk, j:j + 1], bias=modT.t[:, kk, j:j + 1])
                                    else:
                                        stt(o_, pb.t[:, kk % 4, :], A1.t[:, kk, j:j + 1], modT.t[:, kk, j:j + 1].to_broadcast([128, 128]),
                                            ALU.mult, ALU.add, r=[pb.k, A1.k, modT.k], w=[hkk[1]])

                        T1(0)
                        T2(0)
                        for u in range(5):
                            ntile = 4 if u < 4 else 2
                            N = ntile * 128
                            h = hT[u % 2]
                            hkk = hk[u % 2]
                            if u + 1 < 5:
                                T1(u + 1)
                            if _stop(31):
                                return
                            if u < 4:
                                dma("sp", hTs[b, u], h.t[:], r=hkk, w=[T_hTs])
                                tsl = slice(u * 512, u * 512 + N)
                            else:
                                tsl = slice(SEQ, SEQ + N)

                            def proj_fm(c0, Mn=128):
                                nonlocal pcnt
                                P = pP[pcnt % 3]
                                pcnt += 1
                                for kk in range(8):
                                    mm(P.t[0:Mn, 0:N], wk1.t[:, kk, c0:c0 + Mn], h.t[:, kk, 0:N], kk == 0, kk == 7, r=[wk1.k] + hkk, w=[P.k])
                                return P

                            if _stop(32):
                                return
                            for g in range(4):
                                P = proj_fm(512 + g * 128)
                                act(nkT.t[:, g, tsl], P.t[:, 0:N], AF.Identity, r=[P.k, b_nk.k], w=[nkT.k], bias=b_nk.t[:, g:g + 1])
                            if _stop(33):
                                return
                            P = proj_fm(1536 + 128)
                            if u < 4:
                                stt(rbuf.t[:, 0:N], P.t[:, 0:N], b_t4.t[:, 1:2], CS.t[:, tsl], ALU.add, ALU.mult, r=[P.k, b_t4.k, CS.k], w=[rbuf.k])
                                mm(p64.t[0:64, 0:N], stacki.t[:], rbuf.t[:, 0:N], True, True, r=[stacki.k, rbuf.k], w=[p64.k])
                                cp("act", tkT.t[:, tsl], p64.t[0:64, 0:N], r=[p64.k], w=[tkT.k])
                                for g in range(4):
                                    P = proj_fm(g * 128)
                                    act(nqT.t[:, g, tsl], P.t[:, 0:N], AF.Identity, r=[P.k, b_nq.k], w=[nqT.k], bias=b_nq.t[:, g:g + 1])
                                P = proj_fm(1536)
                                stt(rbuf.t[:, 0:N], P.t[:, 0:N], b_t4.t[:, 0:1], CS.t[:, tsl], ALU.add, ALU.mult, r=[P.k, b_t4.k, CS.k], w=[rbuf.k])
                                mm(p64.t[0:64, 0:N], stacki.t[:], rbuf.t[:, 0:N], True, True, r=[stacki.k, rbuf.k], w=[p64.k])
                                cp("act", tqT.t[:, tsl], p64.t[0:64, 0:N], r=[p64.k], w=[tqT.k])
                            else:
                                act(tkT.t[:, tsl], P.t[0:64, 0:N], AF.Identity, r=[P.k, b_t4.k], w=[tkT.k], bias=b_t4.t[0:64, 1:2])
                            if _stop(34):
                                return
                            for tl in range(ntile):
                                kt = (u * 4 + tl) if u < 4 else 16 + tl
                                P = pP[pcnt % 3]
                                pcnt += 1
                                for kk in range(8):
                                    mm(P.t[:, :], h.t[:, kk, tl * 128:(tl + 1) * 128], wk1.t[:, kk, 1024:1536], kk == 0, kk == 7, r=[wk1.k] + hkk, w=[P.k])
                                cp("dve", nva.t[:, kt, :, 0:32], P.t[:, :].rearrange("p (h d) -> p h d", h=16), r=[P.k], w=[nva.k])
                                P = pP[pcnt % 3]
                                pcnt += 1
                                for kk in range(8):
                                    mm(P.t[:, 0:64], h.t[:, kk, tl * 128:(tl + 1) * 128], wk1.t[:, kk, 1792:1856], kk == 0, kk == 7, r=[wk1.k] + hkk, w=[P.k])
                                cp("act", tva.t[:, kt, 0:64], P.t[:, 0:64], r=[P.k], w=[tva.k])
                            if u + 1 < 5:
                                T2(u + 1)
                    S.barrier()
                    if _stop(3):
                        return
                    with ExitStack() as e1b:
                        EB = sb(e1b, "EB", [128, 4, 9, 512], BF16)
                        nst = [sb(e1b, f"nst{i}", [128, 4, 128], F32) for i in range(2)]
                        nmk = load(e1b, "nmk", [128, 9, 128], F32, L["nmk2"].rearrange("t k q -> k t q"))
                        if b == 0:
                            for g in range(4):
                                for ty in range(9):
                                    s_ = (g * 9 + ty) % 2
                                    dma("sp", nst[s_].t[:], L["nab2"][g, ty], r=[], w=[nst[s_].k])
                                    for hh in range(4):
                                        tt("dve", nst[s_].t[:, hh, :], nst[s_].t[:, hh, :], nmk.t[:, ty, :], ALU.add, r=[nst[s_].k, nmk.k], w=[nst[s_].k])
                                    act(EB.t[:, g, ty, :], nst[s_].t[:].rearrange("p a b -> p (a b)"), AF.Exp, r=[nst[s_].k], w=[EB.k])
                            if NB > 1:
                                dma("sp", L["EBs"], EB.t[:], r=[EB.k], w=[L["T_EBs"]])
                        else:
                            dma("sp", EB.t[:], L["EBs"], r=[L["T_EBs"]], w=[EB.k])
                        nqm = [sb(e1b, f"nqm{i}", [128, 4, 128], BF16) for i in range(NAD)]
                        Pn = [sb(e1b, f"Pn{i}", [128, 7, 512], BF16) for i in range(NAD)]
                        Pk = [[S.tok() for _ in range(7)] for _ in range(NAD)]
                        on = sb(e1b, "on", [128, 128], BF16)
                        rec = sb(e1b, "rec", [128, 4], F32)
                        pS = [ps(e1b, f"pS{i}", [128, 512]) for i in range(4)]
                        pO = [ps(e1b, f"pO{i}", [128, 512]) for i in range(2)]
                        pTr = ps(e1b, "pTr", [128, 1024], BF16)
                        scnt = [0]

                        def tiles(i):
                            if 2 <= i <= 13:
                                lat = [(i - 2, 7), (i - 1, 2), (i, 3), (i + 1, 4), (i + 2, 8)]
                            elif i < 2:
                                lat = [(j_, j_ - i + 3) for j_ in range(4)]
                            else:
                                lat = [(j_, j_ - i + 3) for j_ in range(12, 16)]
                            return lat + [(16, None), (17, None)]

                        def QK(n):
                            i, g = n // 4, n % 4
                            nq_ = nqm[n % NAD]
                            for hh in range(4):
                                ts("dve", nq_.t[:, hh, :], nqT.t[:, g, i * 128:(i + 1) * 128], hmask.t[:, hh:hh + 1], ALU.mult,
                                   r=[nqT.k, hmask.k], w=[nq_.k])
                            for ti, (kt, ty) in enumerate(tiles(i)):
                                S_ = pS[scnt[0] % 4]
                                scnt[0] += 1
                                mm(S_.t[:, :], nkT.t[:, g, kt * 128:(kt + 1) * 128], nq_.t[:].rearrange("p a b -> p (a b)"), True, True,
                                   r=[nkT.k, nq_.k], w=[S_.k])
                                pk = Pk[n % NAD][ti]
                                act(Pn[n % NAD].t[:, ti, :], S_.t[:, :], AF.Exp, r=[S_.k], w=[pk], scale=NA_SCALE)
                                if ty is not None:
                                    tt("dve", Pn[n % NAD].t[:, ti, :], Pn[n % NAD].t[:, ti, :], EB.t[:, g, ty, :], ALU.mult, r=[pk, EB.k], w=[pk])

                        def PV(n):
                            i, g = n // 4, n % 4
                            tl_ = tiles(i)
                            nt = len(tl_)
                            pO_ = pO[n % 2]
                            for hh in range(4):
                                h_ = 4 * g + hh
                                for ti, (kt, ty) in enumerate(tl_):
                                    mm(pO_.t[:, hh * 33:(hh + 1) * 33], Pn[n % NAD].t[:, ti, hh * 128:(hh + 1) * 128], nva.t[:, kt, h_, :], ti == 0, ti == nt - 1,
                                       r=[Pk[n % NAD][ti], nva.k], w=[pO_.k])
                            S.op("dve", lambda e: e.reciprocal(out=rec.t[:, 0:4], in_=pO_.t[:, 0:132].rearrange("p (h d) -> p h d", h=4)[:, :, 32]),
                                 r=[pO_.k], w=[rec.k])
                            for hh in range(4):
                                ts("dve", on.t[:, hh * 32:(hh + 1) * 32], pO_.t[:, hh * 33:hh * 33 + 32], rec.t[:, hh:hh + 1], ALU.mult,
                                   r=[pO_.k, rec.k], w=[on.k])
                            tr(pTr.t[:, 0:128], on.t[:], identb.t[:], r=[on.k, identb.k], w=[pTr.k])
                            act(obT.t[:, g, i * 128:(i + 1) * 128], pTr.t[:, 0:128], AF.Identity, r=[pTr.k, b_nv.k], w=[obT.k], bias=b_nv.t[:, g:g + 1])

                        for n in range(NAD - 1):
                            QK(n)
                        for n in range(64):
                            if n + NAD - 1 < 64:
                                QK(n + NAD - 1)
                            PV(n)
                    S.barrier()
                if _stop(4):
                    return
                with ExitStack() as e2:
                    wst = [sb(e2, f"wst{i}", [128, 8, 512], BF16) for i in range(NWST)]
                    wub = sb(e2, "wub", [128, 4, D], BF16)
                    dma("pool", wub.t[:], L["w_up_b"].rearrange("(k p) n -> p k n", p=128), r=[], w=[wub.k])
                    wcnt = [0]

                    def ld(src):
                        wb_ = wst[wcnt[0] % NWST]
                        wcnt[0] += 1
                        if not (NOLD and wcnt[0] > NWST):
                            dma("pool", wb_.t[:], src, r=[], w=[wb_.k])
                        return wb_

                    uT = sb(e2, "uT", [128, 8, 512], BF16)
                    mgT = sb(e2, "mgT", [128, 8, 512], BF16)
                    hT2 = [sb(e2, f"hT2{i}", [128, 8, 512], BF16) for i in range(2)]
                    xs2 = [sb(e2, f"xs2{i}", [128, D], F32) for i in range(2)]
                    vb = sb(e2, "vb", [128, D], F32)
                    vhat = [sb(e2, f"vhat{i}", [128, D], BF16) for i in range(2)]
                    st6 = sb(e2, "st6", [128, 2, 6], F32)
                    mv = sb(e2, "mv", [128, 4], F32)
                    Ptc = [sb(e2, f"Ptc{i}", [128, 18, 128], BF16) for i in range(2)]
                    Ptk = [[S.tok() for _ in range(5)] for _ in range(2)]
                    rc1 = sb(e2, "rc1", [128, 2], F32)
                    tn = sb(e2, "tn", [128, 64], F32)
                    tinyT = [sb(e2, f"tinyT{i}", [64, 128], BF16) for i in range(2)]
                    gab = [sb(e2, f"gab{i}", [128, 512], F32) for i in range(2)]
                    t12 = [sb(e2, f"t12{i}", [128, 512], F32) for i in range(2)]
                    tD = sb(e2, "tD", [128, D], F32)
                    xn2 = sb(e2, "xn2", [128, D], F32)
                    junk2 = sb(e2, "junk2", [128, D], BF16)
                    ss2 = sb(e2, "ss2", [128, 4], F32)
                    h2f2 = [sb(e2, f"h2f{i}", [128, 8, 128], F32) for i in range(2)]
                    h2fk2 = [[S.tok(), S.tok()] for _ in range(2)]
                    h2b = sb(e2, "h2b", [128, 8, 128], BF16)
                    rt2 = [sb(e2, f"rt{i}", [128, 5, NE], F32) for i in range(2)]
                    m82 = [sb(e2, f"m8{i}", [128, 16], F32) for i in range(2)]
                    Q = [ps(e2, f"Q{i}", [128, 512]) for i in range(8)]
                    q4 = lambda q_: q_.t[:, :].rearrange("p (a b) -> p a b", a=4)
                    for s_ in range(4):
                        h = hT2[s_ % 2]
                        dma("sp", h.t[:], hTs[b, s_], r=[T_hTs], w=[h.k])
                        for half in range(2):
                            wb_ = ld(kcols(half * 512, (half + 1) * 512))
                            for f4 in range(4):
                                ft = half * 4 + f4
                                P = Q[ft % 4]
                                for kk in range(8):
                                    mm(P.t[:, :], wb_.t[:, kk, f4 * 128:(f4 + 1) * 128], h.t[:, kk, :], kk == 0, kk == 7, r=[wb_.k, h.k], w=[P.k])
                                act(uT.t[:, ft, :], P.t[:, :], AF.Gelu, r=[P.k, b_u.k], w=[uT.k], bias=b_u.t[:, ft:ft + 1])
                        if _stop(5):
                            return
                        wv = [ld(kcols(1024, 1536)), ld(kcols(1536, 2048))]

                        def XB(c):
                            tq_ = s_ * 4 + c
                            sl_ = c % 2
                            for half in range(2):
                                P = Q[half]
                                for kk in range(8):
                                    mm(P.t[:, :], h.t[:, kk, c * 128:(c + 1) * 128], wv[half].t[:, kk, :], kk == 0, kk == 7, r=[h.k, wv[half].k], w=[P.k])
                                tt("dve", vb.t[:, half * 512:(half + 1) * 512], P.t[:, :], bv_bc.t[:, half * 512:(half + 1) * 512], ALU.add,
                                   r=[P.k, bv_bc.k], w=[vb.k])
                            for grp in range(5):
                                kts = list(range(4 * grp, min(4 * grp + 4, 18)))
                                pSt = Q[2 + grp % 2]
                                for jj, kt in enumerate(kts):
                                    mm(q4(pSt)[:, jj, :], tkT.t[:, kt * 128:(kt + 1) * 128], tqT.t[:, tq_ * 128:(tq_ + 1) * 128], True, True,
                                       r=[tkT.k, tqT.k], w=[pSt.k])
                                act(Ptc[sl_].t[:, kts[0]:kts[-1] + 1, :], q4(pSt)[:, 0:len(kts), :], AF.Exp, r=[pSt.k], w=[Ptk[sl_][grp]], scale=TINY_SCALE)
                            act(vb.t[:], vb.t[:], AF.Gelu, r=[vb.k], w=[vb.k])
                            for i2 in range(2):
                                S.op("dve", lambda e, i2=i2: e.bn_stats(out=st6.t[:, i2, :], in_=vb.t[:, i2 * 512:(i2 + 1) * 512]), r=[vb.k], w=[st6.k])
                            S.op("dve", lambda e: e.bn_aggr(out=mv.t[:, 0:2], in_=st6.t[:].rearrange("p a b -> p (a b)")), r=[st6.k], w=[mv.k])
                            kb.rstd2(mv.t[:, 2:3], mv.t[:, 1:2], mv.t[:, 3:4], [mv.k], [mv.k], 1.0)
                            stt(vb.t[:], vb.t[:], mv.t[:, 0:1], lng_bc.t[:], ALU.subtract, ALU.mult, r=[vb.k, mv.k, lng_bc.k], w=[vb.k])
                            act(vhat[sl_].t[:], vb.t[:], AF.Identity, r=[vb.k, mv.k], w=[vhat[sl_].k], scale=mv.t[:, 3:4])
                            for kt in range(18):
                                mm(Q[4].t[:, 0:65], Ptc[sl_].t[:, kt, :], tva.t[:, kt, :], kt == 0, kt == 17, r=[Ptk[sl_][kt // 4], tva.k], w=[Q[4].k])
                            S.op("dve", lambda e: e.reciprocal(out=rc1.t[:, 0:1], in_=Q[4].t[:, 64:65]), r=[Q[4].k], w=[rc1.k])
                            ts("dve", tn.t[:], Q[4].t[:, 0:64], rc1.t[:, 0:1], ALU.mult, r=[Q[4].k, rc1.k], w=[tn.k])
                            tr(Q[5].t[0:64, 0:128], tn.t[:], ident.t[:], r=[tn.k, ident.k], w=[Q[5].k])
                            act(tinyT[sl_].t[:], Q[5].t[0:64, 0:128], AF.Identity, r=[Q[5].k, b_tv.k], w=[tinyT[sl_].k], bias=b_tv.t[:, 0:1])

                        def YB(c):
                            sl_ = c % 2
                            for ft in range(8):
                                bank = Q[6 + ft // 4]
                                sl = q4(bank)[:, ft % 4, :]
                                mm(sl, vhat[sl_].t[:, ft * 128:(ft + 1) * 128], w_sT.t[:, ft, :], True, False, r=[vhat[sl_].k, w_sT.k], w=[bank.k])
                                mm(sl, two.t[:, ft * 128:(ft + 1) * 128], tinyT[sl_].t[:], False, False, r=[two.k, tinyT[sl_].k], w=[bank.k])
                                mm(sl, LB.t[:, ft * 128:(ft + 1) * 128], RB.t[:, ft * 128:(ft + 1) * 128], False, True, r=[LB.k, RB.k], w=[bank.k])
                            for j2 in range(2):
                                tt("dve", uT.t[:, 4 * j2:4 * j2 + 4, c * 128:(c + 1) * 128], uT.t[:, 4 * j2:4 * j2 + 4, c * 128:(c + 1) * 128],
                                   q4(Q[6 + j2]), ALU.mult, r=[uT.k, Q[6 + j2].k], w=[uT.k])

                        XB(0)
                        for c in range(4):
                            if c + 1 < 4:
                                XB(c + 1)
                            YB(c)
                        if _stop(6):
                            return
                        for half in range(2):
                            wa_ = ld(L["w_up_a"][:, half * 512:(half + 1) * 512].rearrange("(k p) n -> p k n", p=128))
                            wga = ld(kcols(3776 + half * 512, 3776 + (half + 1) * 512))
                            wgb = ld(kcols(4800 + half * 512, 4800 + (half + 1) * 512))
                            for f4 in range(4):
                                ft = half * 4 + f4
                                cs_ = slice(f4 * 128, (f4 + 1) * 128)
                                o4 = 4 * (ft % 2)
                                pA, pB, pGa, pGb = Q[o4], Q[o4 + 1], Q[o4 + 2], Q[o4 + 3]
                                for kk in range(8):
                                    mm(pGa.t[:, :], wga.t[:, kk, cs_], h.t[:, kk, :], kk == 0, kk == 7, r=[wga.k, h.k], w=[pGa.k])
                                for kk in range(8):
                                    mm(pGb.t[:, :], wgb.t[:, kk, cs_], h.t[:, kk, :], kk == 0, kk == 7, r=[wgb.k, h.k], w=[pGb.k])
                                for kk in range(8):
                                    mm(pA.t[:, :], wa_.t[:, kk, cs_], uT.t[:, kk, :], kk == 0, kk == 7, r=[wa_.k, uT.k], w=[pA.k])
                                for kk in range(4):
                                    mm(pB.t[:, :], wub.t[:, kk, ft * 128:(ft + 1) * 128], obT.t[:, kk, s_ * 512:(s_ + 1) * 512], kk == 0, kk == 3,
                                       r=[wub.k, obT.k], w=[pB.k])
                                act(gab[0].t[:], pGa.t[:, :], AF.Sigmoid, r=[pGa.k, b_ga.k], w=[gab[0].k], bias=b_ga.t[:, ft:ft + 1])
                                act(gab[1].t[:], pGb.t[:, :], AF.Sigmoid, r=[pGb.k, b_gb.k], w=[gab[1].k], bias=b_gb.t[:, ft:ft + 1])
                                tt("dve", t12[0].t[:], gab[0].t[:], pA.t[:, :], ALU.mult, r=[gab[0].k, pA.k], w=[t12[0].k])
                                tt("dve", t12[1].t[:], gab[1].t[:], pB.t[:, :], ALU.mult, r=[gab[1].k, pB.k], w=[t12[1].k])
                                tt("dve", mgT.t[:, ft, :], t12[0].t[:], t12[1].t[:], ALU.add, r=[t12[0].k, t12[1].k], w=[mgT.k])
                        if _stop(7):
                            return
                        wo = [ld(L["w_out"][:, hf * 512:(hf + 1) * 512].rearrange("(k p) n -> p k n", p=128)) for hf in range(2)]
                        def D1(c):
                            tq_ = s_ * 4 + c
                            row0 = b * SEQ + tq_ * 128
                            xb_ = xs2[c % 2]
                            h2f = h2f2[c % 2]
                            h2fk = h2fk2[c % 2]
                            dma("sp", xb_.t[:], x[b, tq_ * 128:(tq_ + 1) * 128, :], r=[], w=[xb_.k])
                            for hf in range(2):
                                Qo = Q[hf] if c % 2 == 0 else Q[5 + hf]
                                for kk in range(8):
                                    mm(Qo.t[:, :], mgT.t[:, kk, c * 128:(c + 1) * 128], wo[hf].t[:, kk, :], kk == 0, kk == 7, r=[mgT.k, wo[hf].k], w=[Qo.k])
                                tt("dve", tD.t[:, hf * 512:(hf + 1) * 512], Qo.t[:, :], gmix.t[:, hf * 512:(hf + 1) * 512], ALU.mult,
                                   r=[Qo.k, gmix.k], w=[tD.k])
                            tt("dve", xb_.t[:], xb_.t[:], tD.t[:], ALU.add, r=[xb_.k, tD.k], w=[xb_.k])
                            dma("sp", x1s[row0:row0 + 128, :], xb_.t[:], r=[xb_.k], w=[T_x1s])
                            S.op("dve", lambda e: e.memset(ss2.t[:, 0:1], 0.0), w=[ss2.k])
                            act(junk2.t[:], xb_.t[:], AF.Square, r=[xb_.k], w=[junk2.k, ss2.k], accum=ss2.t[:, 0:1])
                            kb.rstd2(ss2.t[:, 1:2], ss2.t[:, 0:1], ss2.t[:, 2:3], [ss2.k], [ss2.k], 1.0 / D)
                            act(xn2.t[:], xb_.t[:], AF.Identity, r=[xb_.k, ss2.k], w=[xn2.k], scale=ss2.t[:, 2:3])
                            for kk in range(8):
                                pb = Q[2 + kk // 4]
                                tr(q4(pb)[:, kk % 4, :], xn2.t[:, kk * 128:(kk + 1) * 128], ident.t[:], r=[xn2.k, ident.k], w=[pb.k])
                            for kk in range(8):
                                pb = Q[2 + kk // 4]
                                if kk < 4:
                                    act(h2f.t[:, kk, :], q4(pb)[:, kk % 4, :], AF.Identity, r=[pb.k, A2.k, modT.k], w=[h2fk[0]],
                                        scale=A2.t[:, kk, b:b + 1], bias=modT.t[:, 24 + kk, b:b + 1])
                                else:
                                    stt(h2f.t[:, kk, :], q4(pb)[:, kk % 4, :], A2.t[:, kk, b:b + 1], modT.t[:, 24 + kk, b:b + 1].to_broadcast([128, 128]),
                                        ALU.mult, ALU.add, r=[pb.k, A2.k, modT.k], w=[h2fk[1]])
                            cp("act", h2b.t[:], h2f.t[:], r=h2fk, w=[h2b.k])
                            dma("sp", h2s[b, :, :, tq_ * 128:(tq_ + 1) * 128], h2b.t[:], r=[h2b.k], w=[T_h2s])

                        def D2(c):
                            tq_ = s_ * 4 + c
                            row0 = b * SEQ + tq_ * 128
                            h2f = h2f2[c % 2]
                            h2fk = h2fk2[c % 2]
                            rt = rt2[c % 2]
                            m8 = m82[c % 2]
                            for kk in range(8):
                                mm(Q[4].t[:, 0:NE], h2f.t[:, kk, :], wr.t[:, kk, :], kk == 0, kk == 7, r=h2fk + [wr.k], w=[Q[4].k])
                            act(rt.t[:, 0, :], Q[4].t[:, 0:NE], AF.Sigmoid, r=[Q[4].k], w=[rt.k])
                            tt("dve", rt.t[:, 1, :], rt.t[:, 0, :], brt_bc.t[:], ALU.add, r=[rt.k, brt_bc.k], w=[rt.k])
                            S.op("dve", lambda e: e.max(out=m8.t[:, 0:8], in_=rt.t[:, 1, :]), r=[rt.k], w=[m8.k])
                            ts("dve", rt.t[:, 2, :], rt.t[:, 1, :], m8.t[:, 7:8], ALU.is_ge, r=[rt.k, m8.k], w=[rt.k])
                            tt("dve", rt.t[:, 3, :], rt.t[:, 2, :], rt.t[:, 0, :], ALU.mult, r=[rt.k], w=[rt.k])
                            S.op("dve", lambda e: e.reduce_sum(out=m8.t[:, 8:9], in_=rt.t[:, 3, :], axis=AXX), r=[rt.k], w=[m8.k])
                            S.op("dve", lambda e: e.reciprocal(out=m8.t[:, 9:10], in_=m8.t[:, 8:9]), r=[m8.k], w=[m8.k])
                            ts("dve", rt.t[:, 4, :], rt.t[:, 3, :], m8.t[:, 9:10], ALU.mult, r=[rt.k, m8.k], w=[rt.k], s2=2.5, op1=ALU.mult)
                            dma("sp", Gs[row0:row0 + 128, :], rt.t[:, 4, :], r=[rt.k], w=[T_Gs])

                        D1(0)
                        for c in range(4):
                            if c + 1 < 4:
                                D1(c + 1)
                            D2(c)
                        if _stop(8) and s_ == KSS:
                            return
                S.barrier()
        S.barrier()


def _prep_common(inp):
    f = lambda a: np.ascontiguousarray(a, dtype=np.float32)
    fm = lambda v: f(v.reshape(-1, 128).T)
    w_in0 = inp["w_in"][0]
    b_in0 = inp["b_in"][0]
    t = np.arange(64)
    partner = (t // 32) * 32 + (1 - (t % 32) // 16) * 16 + (t % 16)
    TQ, TK, TV, NQ, NK, NV, GA, GB = 2048, 2112, 2176, 2240, 2752, 3264, 3776, 4800
    t4_cols = np.concatenate([TQ + t, TQ + partner, TK + t, TK + partner])
    w_in_ext = np.concatenate([w_in0, w_in0[:, t4_cols]], axis=1)
    b_t4 = b_in0[t4_cols].reshape(2, 128).T
    rpb = inp["na_rpb"][0]
    qr = np.arange(128) // 64
    qc = np.arange(128) % 64
    deltas = [-3, -2, -1, 0, 1, 2, 3, -2, 2]
    c_start = np.clip(qc - 8, 0, 48)
    nabs, masks = [], []
    for ti, dl in enumerate(deltas):
        dr = 2 * dl + qr[None, :] - qr[:, None]
        dc = qc[None, :] - qc[:, None]
        colok = (qc[None, :] >= c_start[:, None]) & (qc[None, :] < c_start[:, None] + 16)
        rowok = np.abs(dr) <= 7
        if ti >= 7:
            rowok = (dr >= -4) & (dr <= 3)
        ok = colok & rowok
        ai = np.clip(dr + 7, 0, 14)
        bi = np.clip(dc + 15, 0, 30)
        nabs.append(rpb[:, ai, bi])
        masks.append(np.where(ok, 0.0, NEG))
    nabs = np.stack(nabs, axis=1)
    nab2 = f(nabs.reshape(4, 4, 9, 128, 128).transpose(0, 2, 4, 1, 3))
    nmk2 = f(np.stack(masks, axis=0).transpose(0, 2, 1))
    tok = np.arange(SEQ)
    pos = np.stack([tok // 64, tok % 64], -1).astype(np.float32)
    inv_freq = (10000.0 ** (-np.arange(16, dtype=np.float32) / 16)).astype(np.float32)
    ang = pos[:, :, None] * inv_freq
    a_idx, h_idx, f_idx = t // 32, (t % 32) // 16, t % 16
    cosT = np.cos(ang)[:, a_idx, f_idx].T
    sinT = np.sin(ang)[:, a_idx, f_idx].T * np.where(h_idx == 0, -1.0, 1.0)[:, None]
    cs = f(np.concatenate([cosT, sinT], 0))
    hm = np.zeros((128, 4), np.float32)
    for hh in range(4):
        hm[32 * hh:32 * hh + 32, hh] = 1.0
    stacki = np.concatenate([np.eye(64), np.eye(64)], 0).astype(np.float32)
    epl = np.zeros((32, 4, 128), np.float32)
    for hh in range(4):
        epl[np.arange(32), hh, 32 * hh + np.arange(32)] = 1.0
    w_gu = np.concatenate([np.concatenate([inp["w_e_gate"][0], inp["w_e_up"][0]], axis=2),
                           np.concatenate([inp["w_sh_gate"][0], inp["w_sh_up"][0]], axis=1)[None]], axis=0)
    w_d = np.concatenate([inp["w_e_down"][0], inp["w_sh_down"][0][None]], axis=0)
    return dict(
        w_ada=f(inp["w_ada"][0]), b_ada=f(inp["b_ada"][0][None]), nmg=fm(inp["norm_mix_g"][0]), nfg=fm(inp["norm_ffn_g"][0]),
        w_in=f(w_in_ext), b_u=fm(b_in0[0:1024]), b_ga=fm(b_in0[GA:GB]), b_gb=fm(b_in0[GB:5824]),
        b_nq=fm(b_in0[NQ:NK]), b_nk=fm(b_in0[NK:NV]), b_nv=fm(b_in0[NV:GA]), b_t4=f(b_t4), b_tv=f(b_in0[TV:NQ][:, None]),
        b_v=f(b_in0[1024:2048][None]), ln_g=f(inp["sgu_ln_g"][0][None]), ln_b=f(inp["sgu_ln_b"][0][None]),
        w_s=f(inp["sgu_w_s"][0]), b_s=f(inp["sgu_b_s"][0].reshape(1, -1)), tiny_w_o=f(inp["tiny_w_o"][0]),
        nab2=nab2, nmk2=nmk2, cs=cs, hmask=hm, stacki=stacki, eplace=epl,
        w_up_a=f(inp["w_up_a"][0]), w_up_b=f(inp["w_up_b"][0]), w_out=f(inp["w_out"][0]),
        w_router=f(inp["w_router"][0]), b_router=f(inp["b_router"][0][None]),
        w_gu=f(w_gu), w_d=f(w_d), fng=f(inp["final_norm_g"][None]),
    )


def _core_inputs(inp, common, core, NB):
    b0 = core * NB
    cc = np.concatenate([inp["c"][b0:b0 + NB], inp["c_ctx"][None]], 0)
    if NB < 4:
        cc = np.concatenate([cc[:NB], np.zeros((4 - NB, D), np.float32), cc[NB:]], 0)
    cT = np.ascontiguousarray(cc.T.reshape(8, 128, 5).transpose(1, 0, 2), dtype=np.float32)
    m = dict(common)
    m.update(x=np.ascontiguousarray(inp["x"][b0:b0 + NB]), ctx=np.ascontiguousarray(inp["ctx"][b0:b0 + NB]), cT=cT)
    return m


_NC_CACHE = {}


def kernel(**inputs):
    inp = {k: np.asarray(v) for k, v in inputs.items()}
    NB = 4
    if "nc" not in _NC_CACHE:
        _NC_CACHE["nc"] = build(NB)
    nc = _NC_CACHE["nc"]
    common = _prep_common(inp)
    in_maps = [_core_inputs(inp, common, c, NB) for c in range(8)]
    res = run_bass_kernel_spmd(nc, in_maps, core_ids=list(range(8)))
    return np.concatenate([r["y"] for r in res.results], axis=0).astype(np.float32)
```
